# Optimizing a Trainium2 kernel written in Bass

```python
import math
import jax, jax.numpy as jnp
from jax import lax
import numpy as np

D_MODEL = 1024
BATCH = 32
SEQ = 2048
DEPTH = 4

NSA_HEADS = 8
NSA_KV_GROUPS = 2
NSA_HEAD_DIM = 64
NSA_REP = NSA_HEADS // NSA_KV_GROUPS
NSA_WIDTH = NSA_HEADS * NSA_HEAD_DIM
KV_WIDTH = NSA_KV_GROUPS * NSA_HEAD_DIM
CMP_BLOCK = 32
CMP_STRIDE = 16
CMP_HIDDEN = 128
SLC_BLOCK = 64
SLC_TOP = 16
SLC_QBLOCK = 16
WIN = 512
WIN_QBLOCK = 128
SSD_HEADS = 8
SSD_HEAD_DIM = 64
SSD_WIDTH = SSD_HEADS * SSD_HEAD_DIM
SSD_GROUPS = 2
SSD_STATE = 128
SSD_CONV = 4
SSD_CHUNK = 256
SSD_XBC = SSD_WIDTH + 2 * SSD_GROUPS * SSD_STATE
D_MIX = NSA_WIDTH + SSD_WIDTH
D_FF = 2816
FFN_CONV = 3
RMS_EPS = 1e-6
NEG = -1e30
FORCE_BONUS = 1e4

IN_SIZES = [NSA_WIDTH, KV_WIDTH, KV_WIDTH, KV_WIDTH, KV_WIDTH, KV_WIDTH, KV_WIDTH,
            3 * NSA_HEADS, SSD_WIDTH, SSD_XBC, SSD_HEADS]
IN_COLS = sum(IN_SIZES)
IN_SPLITS = [int(v) for v in np.cumsum(IN_SIZES)[:-1]]

kernel_name = "hymba_nsa_ssd_convffn"


def _rmsnorm(x, w):
    xf = x.astype(jnp.float32)
    y = xf * lax.rsqrt(jnp.mean(xf * xf, axis=-1, keepdims=True) + RMS_EPS)
    return (y * w).astype(x.dtype)


def _causal_dwconv(x, w, b):
    k, c = w.shape
    y = lax.conv_general_dilated(x, w[:, None, :], window_strides=(1,), padding=[(k - 1, 0)],
                                 dimension_numbers=('NWC', 'WIO', 'NWC'), feature_group_count=c)
    return y + b


def _masked_softmax(s, mask):
    p = jax.nn.softmax(jnp.where(mask, s, NEG), axis=-1)
    return jnp.where(mask, p, 0.0)


def _compress(k, pos, w1, b1, w2):
    b, s, g, d = k.shape
    nc = (s - CMP_BLOCK) // CMP_STRIDE + 1
    idx = np.arange(nc)[:, None] * CMP_STRIDE + np.arange(CMP_BLOCK)[None, :]
    kb = k[:, idx] + pos[:, None, :]
    kb = jnp.moveaxis(kb, 2, 3).reshape(b, nc, g, CMP_BLOCK * d)
    return jax.nn.gelu(kb @ w1 + b1) @ w2


def _select_blocks(p_cmp, s):
    nc = p_cmp.shape[-1]
    ns = s // SLC_BLOCK
    c0 = np.arange(nc)[:, None] * CMP_STRIDE
    j0 = np.arange(ns)[None, :] * SLC_BLOCK
    overlap = jnp.asarray(((c0 < j0 + SLC_BLOCK) & (c0 + CMP_BLOCK > j0)).astype(np.float32))
    imp = jnp.einsum('bgrsc,cj->bgsj', p_cmp, overlap)
    cur = np.arange(s)[:, None] // SLC_BLOCK
    jb = np.arange(ns)[None, :]
    valid = jb <= cur
    forced = ((jb == 0) | (jb == cur) | (jb == cur - 1)).astype(np.float32)
    score = jnp.where(valid, imp + FORCE_BONUS * forced, NEG)
    _, idx = lax.top_k(score, min(SLC_TOP, ns))
    return idx


def _selected_attn(q, k, v, blk_idx):
    b, s, g, r, d = q.shape
    ns = s // SLC_BLOCK
    n = blk_idx.shape[-1]
    nq = s // SLC_QBLOCK
    scale = d ** -0.5
    kb = k.reshape(b, ns, SLC_BLOCK, g, d).transpose(0, 3, 1, 2, 4)
    vb = v.reshape(b, ns, SLC_BLOCK, g, d).transpose(0, 3, 1, 2, 4)
    qs = jnp.moveaxis(q.reshape(b, nq, SLC_QBLOCK, g, r, d), 1, 0)
    ixs = jnp.moveaxis(blk_idx.reshape(b, g, nq, SLC_QBLOCK, n), 2, 0)
    ts = jnp.arange(s).reshape(nq, SLC_QBLOCK)
    gather = jax.vmap(jax.vmap(lambda blocks, ix: blocks[ix]))

    def block(args):
        q_i, ix, t = args
        k_i = gather(kb, ix)
        v_i = gather(vb, ix)
        sc = jnp.einsum('bqgrd,bgqnld->bgrqnl', q_i, k_i).astype(jnp.float32) * scale
        key_pos = ix[..., None] * SLC_BLOCK + jnp.arange(SLC_BLOCK)
        mask = (key_pos <= t[:, None, None])[:, :, None]
        p = _masked_softmax(sc.reshape(b, g, r, SLC_QBLOCK, n * SLC_BLOCK),
                            mask.reshape(b, g, 1, SLC_QBLOCK, n * SLC_BLOCK))
        p = p.reshape(b, g, r, SLC_QBLOCK, n, SLC_BLOCK).astype(v.dtype)
        return jnp.einsum('bgrqnl,bgqnld->bqgrd', p, v_i)

    o = lax.map(block, (qs, ixs, ts))
    return jnp.moveaxis(o, 0, 1).reshape(b, s, g, r, d)


def _window_attn(q, k, v):
    b, s, g, r, d = q.shape
    nq = s // WIN_QBLOCK
    span = WIN_QBLOCK + WIN
    scale = d ** -0.5
    kp = jnp.pad(k, ((0, 0), (WIN, 0), (0, 0), (0, 0)))
    vp = jnp.pad(v, ((0, 0), (WIN, 0), (0, 0), (0, 0)))
    rel = np.arange(WIN_QBLOCK)[:, None] + WIN - np.arange(span)[None, :]
    band = (rel >= 0) & (rel < WIN)

    def block(i):
        q_i = lax.dynamic_slice_in_dim(q, i * WIN_QBLOCK, WIN_QBLOCK, axis=1)
        k_i = lax.dynamic_slice_in_dim(kp, i * WIN_QBLOCK, span, axis=1)
        v_i = lax.dynamic_slice_in_dim(vp, i * WIN_QBLOCK, span, axis=1)
        key_pos = i * WIN_QBLOCK - WIN + jnp.arange(span)
        mask = band & (key_pos >= 0)[None, :]
        sc = jnp.einsum('bqgrd,bkgd->bgrqk', q_i, k_i).astype(jnp.float32) * scale
        p = _masked_softmax(sc, mask).astype(v.dtype)
        return jnp.einsum('bgrqk,bkgd->bqgrd', p, v_i)

    o = lax.map(block, jnp.arange(nq))
    return jnp.moveaxis(o, 0, 1).reshape(b, s, g, r, d)


def _nsa(q, kc, vc, ks, vs, kw, vw, gl, pos_k, w1_k, b1_k, w2_k, pos_v, w1_v, b1_v, w2_v, out_norm_w):
    b, s, _ = q.shape
    G, R, dk = NSA_KV_GROUPS, NSA_REP, NSA_HEAD_DIM
    q = q.reshape(b, s, G, R, dk)
    kv = lambda t: t.reshape(b, s, G, dk)
    k_cmp = _compress(kv(kc), pos_k, w1_k, b1_k, w2_k)
    v_cmp = _compress(kv(vc), pos_v, w1_v, b1_v, w2_v)
    nc = k_cmp.shape[1]
    sc = jnp.einsum('bsgrd,bcgd->bgrsc', q, k_cmp).astype(jnp.float32) * dk ** -0.5
    cmp_end = np.arange(nc) * CMP_STRIDE + CMP_BLOCK - 1
    cmask = cmp_end[None, :] <= np.arange(s)[:, None]
    p_cmp = _masked_softmax(sc, cmask)
    o_cmp = jnp.einsum('bgrsc,bcgd->bsgrd', p_cmp.astype(v_cmp.dtype), v_cmp)
    idx = _select_blocks(p_cmp, s)
    o_slc = _selected_attn(q, kv(ks), kv(vs), idx)
    o_win = _window_attn(q, kv(kw), kv(vw))
    gates = jax.nn.sigmoid(gl.astype(jnp.float32)).reshape(b, s, G, R, 3).astype(q.dtype)
    o = gates[..., 0:1] * o_cmp + gates[..., 1:2] * o_slc + gates[..., 2:3] * o_win
    return _rmsnorm(o.reshape(b, s, NSA_WIDTH), out_norm_w)


def _ssd_scan(x, dt, A, Bm, Cm, chunk):
    b, s, h, p = x.shape
    g, n = Bm.shape[2], Bm.shape[3]
    r = h // g
    c = s // chunk
    xf = (x * dt[..., None]).reshape(b, c, chunk, g, r, p)
    a = jnp.moveaxis((dt * A).reshape(b, c, chunk, g, r), 2, -1)
    a_cs = jnp.cumsum(a, axis=-1)
    Bc = Bm.reshape(b, c, chunk, g, n)
    Cc = Cm.reshape(b, c, chunk, g, n)
    tri = np.tril(np.ones((chunk, chunk), dtype=bool))
    seg = a_cs[..., :, None] - a_cs[..., None, :]
    Lmat = jnp.where(tri, jnp.exp(jnp.where(tri, seg, 0.0)), 0.0)
    cb = jnp.einsum('bclgn,bcsgn->bcgls', Cc, Bc)
    y_diag = jnp.einsum('bcgrls,bcsgrp->bclgrp', cb[:, :, :, None] * Lmat, xf)
    decay = jnp.exp(a_cs[..., -1:] - a_cs)
    states = jnp.einsum('bclgn,bcgrl,bclgrp->bcgrpn', Bc, decay, xf)
    chunk_decay = jnp.exp(a_cs[..., -1])

    def step(carry, inp):
        st, dec = inp
        return dec[..., None, None] * carry + st, carry

    init = jnp.zeros_like(states[:, 0])
    _, prev = lax.scan(step, init, (jnp.moveaxis(states, 1, 0), jnp.moveaxis(chunk_decay, 1, 0)))
    prev = jnp.moveaxis(prev, 0, 1)
    y_off = jnp.einsum('bclgn,bcgrpn,bcgrl->bclgrp', Cc, prev, jnp.exp(a_cs))
    return (y_diag + y_off).reshape(b, s, h, p).astype(x.dtype)


def _ssd(z, xbc, dt_raw, conv_w, conv_b, dt_bias, A_log, D, norm_w):
    b, s, _ = z.shape
    xbc = jax.nn.silu(_causal_dwconv(xbc, conv_w, conv_b))
    xs, Bm, Cm = jnp.split(xbc, [SSD_WIDTH, SSD_WIDTH + SSD_GROUPS * SSD_STATE], axis=-1)
    xs = xs.reshape(b, s, SSD_HEADS, SSD_HEAD_DIM)
    Bm = Bm.reshape(b, s, SSD_GROUPS, SSD_STATE)
    Cm = Cm.reshape(b, s, SSD_GROUPS, SSD_STATE)
    dt = jax.nn.softplus((dt_raw + dt_bias).astype(jnp.float32))
    A = -jnp.exp(A_log.astype(jnp.float32))
    y = _ssd_scan(xs, dt, A, Bm, Cm, math.gcd(SSD_CHUNK, s))
    y = y + D[:, None] * xs
    y = y.reshape(b, s, SSD_WIDTH) * jax.nn.silu(z)
    yg = y.reshape(b, s, SSD_GROUPS, SSD_WIDTH // SSD_GROUPS).astype(jnp.float32)
    yg = yg * lax.rsqrt(jnp.mean(yg * yg, axis=-1, keepdims=True) + RMS_EPS)
    return (yg.reshape(b, s, SSD_WIDTH) * norm_w).astype(z.dtype)


def _conv_ffn(h, w_gate, w_up, conv_w, conv_b, w_down):
    gate = _causal_dwconv(h @ w_gate, conv_w, conv_b)
    return (jax.nn.silu(gate) * (h @ w_up)) @ w_down


def setup_inputs(seed: int = 0) -> dict:
    key = jax.random.key(seed)
    ks = jax.random.split(key, 32)
    L = DEPTH
    nrm = lambda k, shape, sc: sc * jax.random.normal(k, shape, jnp.float32)
    dt0 = jnp.exp(jax.random.uniform(ks[14], (L, SSD_HEADS), jnp.float32, math.log(1e-3), math.log(1e-1)))
    return {
        "x": jax.random.normal(ks[0], (BATCH, SEQ, D_MODEL), jnp.float32),
        "norm_mix_w": 1.0 + nrm(ks[1], (L, D_MODEL), 0.02),
        "w_in": nrm(ks[2], (L, D_MODEL, IN_COLS), D_MODEL ** -0.5),
        "cmp_pos_k": nrm(ks[3], (L, CMP_BLOCK, NSA_HEAD_DIM), 0.5),
        "cmp_w1_k": nrm(ks[4], (L, CMP_BLOCK * NSA_HEAD_DIM, CMP_HIDDEN), (CMP_BLOCK * NSA_HEAD_DIM) ** -0.5),
        "cmp_b1_k": nrm(ks[5], (L, CMP_HIDDEN), 0.02),
        "cmp_w2_k": nrm(ks[6], (L, CMP_HIDDEN, NSA_HEAD_DIM), CMP_HIDDEN ** -0.5),
        "cmp_pos_v": nrm(ks[7], (L, CMP_BLOCK, NSA_HEAD_DIM), 0.5),
        "cmp_w1_v": nrm(ks[8], (L, CMP_BLOCK * NSA_HEAD_DIM, CMP_HIDDEN), (CMP_BLOCK * NSA_HEAD_DIM) ** -0.5),
        "cmp_b1_v": nrm(ks[9], (L, CMP_HIDDEN), 0.02),
        "cmp_w2_v": nrm(ks[10], (L, CMP_HIDDEN, NSA_HEAD_DIM), CMP_HIDDEN ** -0.5),
        "nsa_norm_w": 1.0 + nrm(ks[11], (L, NSA_WIDTH), 0.02),
        "ssd_conv_w": nrm(ks[12], (L, SSD_CONV, SSD_XBC), SSD_CONV ** -0.5),
        "ssd_conv_b": nrm(ks[13], (L, SSD_XBC), 0.02),
        "ssd_dt_bias": dt0 + jnp.log(-jnp.expm1(-dt0)),
        "ssd_A_log": jnp.log(jax.random.uniform(ks[15], (L, SSD_HEADS), jnp.float32, 1.0, 16.0)),
        "ssd_D": 1.0 + nrm(ks[16], (L, SSD_HEADS), 0.1),
        "ssd_norm_w": 1.0 + nrm(ks[17], (L, SSD_WIDTH), 0.02),
        "w_out": nrm(ks[18], (L, D_MIX, D_MODEL), D_MIX ** -0.5),
        "norm_ffn_w": 1.0 + nrm(ks[19], (L, D_MODEL), 0.02),
        "w_gate": nrm(ks[20], (L, D_MODEL, D_FF), D_MODEL ** -0.5),
        "w_up": nrm(ks[21], (L, D_MODEL, D_FF), D_MODEL ** -0.5),
        "ffn_conv_w": nrm(ks[22], (L, FFN_CONV, D_FF), FFN_CONV ** -0.5),
        "ffn_conv_b": nrm(ks[23], (L, D_FF), 0.02),
        "w_down": nrm(ks[24], (L, D_FF, D_MODEL), D_FF ** -0.5),
        "norm_final_w": 1.0 + nrm(ks[25], (D_MODEL,), 0.02),
    }


def reference(x, norm_mix_w, w_in, cmp_pos_k, cmp_w1_k, cmp_b1_k, cmp_w2_k, cmp_pos_v, cmp_w1_v,
              cmp_b1_v, cmp_w2_v, nsa_norm_w, ssd_conv_w, ssd_conv_b, ssd_dt_bias, ssd_A_log, ssd_D,
              ssd_norm_w, w_out, norm_ffn_w, w_gate, w_up, ffn_conv_w, ffn_conv_b, w_down, norm_final_w):
    for i in range(DEPTH):
        h = _rmsnorm(x, norm_mix_w[i])
        q, kc, vc, ks_, vs_, kw, vw, gl, z, xbc, dtr = jnp.split(h @ w_in[i], IN_SPLITS, axis=-1)
        o_attn = _nsa(q, kc, vc, ks_, vs_, kw, vw, gl,
                      cmp_pos_k[i], cmp_w1_k[i], cmp_b1_k[i], cmp_w2_k[i],
                      cmp_pos_v[i], cmp_w1_v[i], cmp_b1_v[i], cmp_w2_v[i], nsa_norm_w[i])
        o_ssd = _ssd(z, xbc, dtr, ssd_conv_w[i], ssd_conv_b[i], ssd_dt_bias[i], ssd_A_log[i],
                     ssd_D[i], ssd_norm_w[i])
        x = x + jnp.concatenate([o_attn, o_ssd], axis=-1) @ w_out[i]
        h = _rmsnorm(x, norm_ffn_w[i])
        x = x + _conv_ffn(h, w_gate[i], w_up[i], ffn_conv_w[i], ffn_conv_b[i], w_down[i])
    return _rmsnorm(x, norm_final_w)
```

```python
from contextlib import ExitStack
import numpy as np
import ml_dtypes
import concourse.bass as bass
import concourse.mybir as mybir
from concourse.bass_utils import run_bass_kernel_spmd

F32 = mybir.dt.float32
BF16 = mybir.dt.bfloat16
AF = mybir.ActivationFunctionType
ALU = mybir.AluOpType
AX = mybir.AxisListType


class Res:
    __slots__ = ("t", "name", "lw", "rd")

    def __init__(self, t, name):
        self.t = t
        self.name = name
        self.lw = None
        self.rd = {}

    def sub(self, n):
        return [Res(self.t, "%s_%d" % (self.name, i)) for i in range(n)]


class Prog:
    ENGS = ("pe", "act", "dve", "pool", "sp")

    def __init__(self, nc):
        self.nc = nc
        self.stack = ExitStack()
        self.ops = []
        self.by_eng = {e: [] for e in self.ENGS}
        self.out_ops = []

    def sb(self, name, shape, dt):
        t = self.stack.enter_context(self.nc.sbuf_tensor(name, list(shape), dt))
        return Res(t, name)

    def ps(self, name, shape, dt):
        t = self.stack.enter_context(self.nc.psum_tensor(name, list(shape), dt))
        return Res(t, name)

    def _op(self, eng, fn, r, w, semkey):
        deps = set()
        for x in r:
            if x.lw is not None:
                deps.add(x.lw)
        for x in w:
            if x.lw is not None:
                deps.add(x.lw)
            for v in x.rd.values():
                deps.add(v)
        oid = len(self.ops)
        self.ops.append((eng, fn, deps, semkey))
        self.by_eng[eng].append(oid)
        for x in w:
            x.lw = oid
            x.rd = {}
        for x in r:
            if x not in w:
                x.rd[semkey] = oid
        return oid

    def pe(self, fn, r=(), w=()):
        return self._op("pe", fn, r, w, "pe")

    def act(self, fn, r=(), w=()):
        return self._op("act", fn, r, w, "act")

    def dve(self, fn, r=(), w=()):
        return self._op("dve", fn, r, w, "dve")

    def pool(self, fn, r=(), w=()):
        return self._op("pool", fn, r, w, "pool")

    def _dkey(self, r, w):
        for x in list(w) + list(r):
            if x.t is not None:
                return "dma_" + x.name
        return "dma_misc"

    def dma(self, out, in_, r=(), w=(), q="sp", key=None, out_final=False):
        semkey = self._dkey(r, w)
        oid = self._op(q, lambda e: e.dma_start(out=out, in_=in_), r, w, semkey)
        if out_final:
            self.out_ops.append(oid)
        return oid

    def _op_dma_slow(self, out, in_, r, w, q):
        semkey = self._dkey(r, w)
        return self._op(q, lambda e: e.dma_start(out=out, in_=in_, allow_slow_non_contiguous=True), r, w, semkey)

    def finish(self):
        nc = self.nc
        ops = self.ops
        n = len(ops)
        signal = [False] * n
        for (eng, fn, deps, sk) in ops:
            for d in deps:
                if not (sk == "pe" and ops[d][3] == "pe"):
                    signal[d] = True
        for o in self.out_ops:
            signal[o] = True
        for i, op in enumerate(ops):
            if op[3].startswith("dma_"):
                signal[i] = True
        semcnt = {}
        val = [0] * n
        for i, (eng, fn, deps, sk) in enumerate(ops):
            if signal[i]:
                inc = 16 if sk.startswith("dma_") else 1
                semcnt[sk] = semcnt.get(sk, 0) + inc
                val[i] = semcnt[sk]
        sems = {}
        for sk in semcnt:
            sems[sk] = self.stack.enter_context(nc.semaphore(sk))
        self.semcnt = semcnt
        waited = {e: {} for e in self.ENGS}
        by_eng = self.by_eng
        out_ops = self.out_ops

        def emit(eng, e):
            wd = waited[eng]
            for oid in by_eng[eng]:
                _, fn, deps, sk = ops[oid]
                need = {}
                for d in deps:
                    dsk = ops[d][3]
                    if dsk == "pe" and eng == "pe":
                        continue
                    v = val[d]
                    if v > need.get(dsk, 0):
                        need[dsk] = v
                for dsk, v in need.items():
                    if wd.get(dsk, 0) < v:
                        e.wait_ge(sems[dsk], v)
                        wd[dsk] = v
                ins = fn(e)
                if signal[oid]:
                    ins.then_inc(sems[sk], 16 if sk.startswith("dma_") else 1)
            if eng == "sp":
                need = {}
                for o in out_ops:
                    dsk = ops[o][3]
                    need[dsk] = max(need.get(dsk, 0), val[o])
                for dsk, v in need.items():
                    e.wait_ge(sems[dsk], v)

        with nc.Block() as block:
            @block.tensor
            def _(e):
                emit("pe", e)

            @block.scalar
            def _(e):
                emit("act", e)

            @block.vector
            def _(e):
                emit("dve", e)

            @block.gpsimd
            def _(e):
                emit("pool", e)

            @block.sync
            def _(e):
                emit("sp", e)
        self.stack.close()


S = 2048
D = 1024
NT = 16
L = 4
DFF = 2816
NFF = 22
INC = 2848
NEGB = -30000.0
QT0, KC0, VC0, KS0, KW0, XBC0, DTR0, Z0, VS0, VW0, GL0 = 0, 512, 640, 768, 896, 1024, 2048, 2056, 2568, 2696, 2824
WIN_SEGS = ([((g * 4 + r) * 64, 64) for r in range(4) for g in range(2)] +
            [(512, 128), (640, 128), (768, 128), (1024, 128), (1816, 1024), (2840, 8),
             (1304, 512), (896, 128), (1152, 128), (1280, 24)])


def host_consts():
    bf = ml_dtypes.bfloat16
    c = {}
    c["c_identb"] = np.eye(128, dtype=np.float32).astype(bf)
    c["c_identf"] = np.eye(128, dtype=np.float32)
    p = np.arange(128)[:, None]
    cc = np.arange(512)[None, :]
    causal = np.stack([np.where(cc - p >= 128 * jj, 0.0, NEGB) for jj in range(4)], axis=1)
    c["c_causal"] = causal.astype(np.float32).astype(bf)
    band = np.stack([np.where(cc - p < 128 * (4 - m), 0.0, NEGB) for m in range(1, 5)], axis=1)
    c["c_band"] = band.astype(np.float32).astype(bf)
    t = np.arange(S)[None, :]
    ci = np.arange(128)[:, None]
    c["c_cmpb"] = np.where((16 * ci + 31 <= t) & (ci < 127), 0.0, NEGB).astype(np.float32).astype(bf)
    e = np.zeros((32, 16, 128), np.float32)
    for tl in range(16):
        for pp in range(128):
            e[2 * tl + pp // 64, tl, pp] = 1.0
    c["c_eexp"] = e.astype(bf)
    c0 = np.arange(128)[:, None] * 16
    j0 = np.arange(32)[None, :] * 64
    ov = ((c0 < j0 + 64) & (c0 + 32 > j0)).astype(np.float32)
    ov[127] = 0.0
    c["c_ovl"] = ov
    tt = 1024 + np.arange(1024)[:, None]
    cur = tt // 64
    jb = np.arange(32)[None, :]
    valid = jb <= cur
    forced = ((jb == 0) | (jb == cur) | (jb == cur - 1)).astype(np.float32)
    fs = np.where(valid & (forced < 0.5), 0.0, -1e30).astype(np.float32)
    c["c_fsel"] = np.ascontiguousarray(fs.reshape(8, 128, 32).transpose(1, 0, 2))
    fo = (forced * (-NEGB)).astype(np.float32)
    c["c_forc"] = np.ascontiguousarray(fo.reshape(8, 128, 32).transpose(1, 0, 2))
    tk = np.arange(256)[:, None]
    ll = np.arange(256)[None, :]
    tri = (tk <= ll).astype(np.float32)
    c["c_triu"] = np.ascontiguousarray(tri.reshape(2, 128, 256).transpose(1, 0, 2))
    c["c_tri01"] = (np.arange(128)[None, :] >= np.arange(128)[:, None]).astype(np.float32)
    c["c_ones"] = np.ones((128, 128), np.float32)
    return c


CONST_SHAPES = {
    "c_identb": ([128, 128], BF16), "c_identf": ([128, 128], F32), "c_causal": ([128, 4, 512], BF16),
    "c_band": ([128, 4, 512], BF16), "c_cmpb": ([128, 2048], BF16), "c_eexp": ([32, 16, 128], BF16),
    "c_ovl": ([128, 32], F32), "c_fsel": ([128, 8, 32], F32), "c_forc": ([128, 8, 32], F32), "c_triu": ([128, 2, 256], F32),
    "c_tri01": ([128, 128], F32), "c_ones": ([128, 128], F32),
}

WEIGHT_SHAPES = {
    "norm_mix_w": [L, D], "w_in": [L, D, INC], "cmp_pos_k": [L, 32, 64], "cmp_w1_k": [L, 2048, 128],
    "cmp_b1_k": [L, 128], "cmp_w2_k": [L, 128, 64], "cmp_pos_v": [L, 32, 64], "cmp_w1_v": [L, 2048, 128],
    "cmp_b1_v": [L, 128], "cmp_w2_v": [L, 128, 64], "nsa_norm_w": [L, 512], "ssd_conv_w": [L, 4, 1024],
    "ssd_conv_b": [L, 1024], "ssd_dt_bias": [L, 8], "ssd_A_log": [L, 8], "ssd_D": [L, 8],
    "ssd_norm_w": [L, 512], "w_out": [L, D, D], "norm_ffn_w": [L, D], "w_gate": [L, D, DFF],
    "w_up": [L, D, DFF], "ffn_conv_w": [L, 3, DFF], "ffn_conv_b": [L, DFF], "w_down": [L, DFF, D],
    "norm_final_w": [D],
}


import os as _os0
XQ = "sp"
XQS = _os0.environ.get("MKQS", "act")


class Arena:
    def __init__(self, limit=229344 - 64):
        self.off = 16640 + 64
        self.limit = limit

    def alloc(self, nbytes):
        o = (self.off + 63) // 64 * 64
        self.off = o + nbytes
        self.peak = max(getattr(self, "peak", 0), self.off)
        assert self.off <= self.limit, ("SBUF overflow", self.off)
        return o


def _nbytes(shape, dt):
    n = 1
    for s in shape[1:]:
        n *= s
    return n * (4 if dt == F32 else 2)


class K:
    def __init__(self, nseq=4, nlayer=4, dbg=False):
        self.nseq, self.nlayer, self.dbg = nseq, nlayer, dbg
        nc = self.nc = bass.Bass("TRN2", target_bir_lowering=False)
        self.p = Prog(nc)
        self.ar = Arena()
        self.tog = 0
        d = {}
        d["x"] = nc.dram_tensor("x", [nseq, S, D], F32, kind="ExternalInput").ap()
        for k, shp in WEIGHT_SHAPES.items():
            d[k] = nc.dram_tensor(k, shp, F32, kind="ExternalInput").ap()
        for k, (shp, dt) in CONST_SHAPES.items():
            d[k] = nc.dram_tensor(k, shp, dt, kind="ExternalInput").ap()
        d["out"] = nc.dram_tensor("out", [nseq, S, D], F32, kind="ExternalOutput").ap()
        d["win_s"] = nc.dram_tensor("win_s", [L, 128, 8, INC], BF16, kind="Internal").ap()
        d["wout_s"] = nc.dram_tensor("wout_s", [L, 128, 8, D], BF16, kind="Internal").ap()
        d["wg_s"] = nc.dram_tensor("wg_s", [L, 128, 8, DFF], BF16, kind="Internal").ap()
        d["wu_s"] = nc.dram_tensor("wu_s", [L, 128, 8, DFF], BF16, kind="Internal").ap()
        d["wd_s"] = nc.dram_tensor("wd_s", [L, 128, NFF, D], BF16, kind="Internal").ap()
        d["w1_s"] = nc.dram_tensor("w1_s", [L, 2, 128, 32, 128], BF16, kind="Internal").ap()
        d["xs_s"] = nc.dram_tensor("xs_s", [S, D], F32, kind="Internal").ap()
        self.d = d
        self.ndma = 0

    def sb(self, name, shape, dt):
        off = self.ar.alloc(_nbytes(shape, dt))
        t = self.nc.alloc_sbuf_tensor_at(name, list(shape), dt, offset=off)
        return Res(t, name)

    def mm(self, out, lhsT, rhs, start, stop, r, w):
        self.p.pe(lambda e: e.matmul(out, lhsT, rhs, start=start, stop=stop), r=r, w=w)

    def tr(self, out, in_, ident, r, w):
        self.p.pe(lambda e: e.transpose(out, in_, ident), r=r, w=w)

    def actf(self, out, in_, func, r, w, bias=None, scale=None, accum=None):
        kw = {}
        if bias is not None:
            kw["bias"] = bias
        if scale is not None:
            kw["scale"] = scale
        if accum is not None:
            kw["accum_out"] = accum
        self.p.act(lambda e: e.activation(out, in_, func, **kw), r=r, w=w)

    def cp(self, out, in_, r, w, eng=None):
        if eng is None:
            self.tog ^= 1
            eng = "act" if self.tog else "dve"
        if eng == "act":
            self.p.act(lambda e: e.activation(out, in_, AF.Copy), r=r, w=w)
        elif eng == "dve":
            self.p.dve(lambda e: e.tensor_copy(out, in_), r=r, w=w)
        else:
            self.p.pool(lambda e: e.tensor_copy(out, in_), r=r, w=w)

    def ts(self, out, in0, s1, s2, op0, op1=None, r=(), w=(), eng="dve"):
        if op1 is None:
            f = lambda e: e.tensor_scalar(out, in0, s1, s2, op0)
        else:
            f = lambda e: e.tensor_scalar(out, in0, s1, s2, op0, op1)
        getattr(self.p, eng)(f, r=r, w=w)

    def tt(self, out, in0, in1, op, r=(), w=(), eng="dve"):
        getattr(self.p, eng)(lambda e: e.tensor_tensor(out, in0, in1, op), r=r, w=w)

    def stt(self, out, in0, sc, in1, op0, op1, r=(), w=(), eng="dve"):
        getattr(self.p, eng)(lambda e: e.scalar_tensor_tensor(out, in0, sc, in1, op0, op1), r=r, w=w)

    def dma(self, out, in_, r=(), w=(), out_final=False, slow=False, q="sp"):
        if slow:
            self.p._op_dma_slow(out, in_, r, w, q)
        else:
            self.p.dma(out, in_, r=r, w=w, q=q, out_final=out_final)

    def cv(self, out, in_, scale, r, w):
        self.cvi = getattr(self, "cvi", 0) + 1
        k = self.cvi % 3 if scale is None else self.cvi % 2
        if k == 0:
            if scale is None:
                self.p.act(lambda e: e.activation(out, in_, AF.Copy), r=r, w=w)
            else:
                self.p.act(lambda e: e.activation(out, in_, AF.Copy, scale=scale), r=r, w=w)
        else:
            eng = self.p.dve if k == 1 else self.p.pool
            if scale is None:
                eng(lambda e: e.tensor_copy(out, in_), r=r, w=w)
            else:
                eng(lambda e: e.tensor_scalar(out, in_, scale, None, ALU.mult), r=r, w=w)

    def prologue(self):
        p, d = self.p, self.d
        stg = [self.sb("stg%d" % i, [128, 8192], F32) for i in range(2)]
        stb = [self.sb("stb%d" % i, [128, 8192], BF16) for i in range(2)]
        nw = self.sb("nw", [128, L, 3, 8], F32)
        self.blk = 0

        blocks = []

        def prep(src, KC, N, segs, dst, scale, scale_res):
            CB = min(N, (8192 // KC) // 64 * 64)
            srcv = src.rearrange("(kc p) n -> p kc n", p=128)
            tab = []
            o = 0
            for (sc, ln) in segs:
                tab.append((o, sc, ln))
                o += ln
            assert o == N
            for c0 in range(0, N, CB):
                cb = min(CB, N - c0)
                loads = []
                for (ds, ss, ln) in tab:
                    a, b = max(ds, c0), min(ds + ln, c0 + cb)
                    if a < b:
                        loads.append((a - c0, b - c0, srcv[:, :, ss + (a - ds):ss + (b - ds)]))
                blocks.append((KC, cb, loads, scale, scale_res, dst[:, :, c0:c0 + cb]))

        def emit_load(k):
            KC, cb, loads, scale, scale_res, dst = blocks[k]
            sl = k % 2
            fv = stg[sl].t[:, 0:KC * cb].rearrange("p (kc n) -> p kc n", kc=KC)
            for (a, b, src) in loads:
                self.dma(fv[:, :, a:b], src, w=[stg[sl]])

        def emit_conv(k):
            KC, cb, loads, scale, scale_res, dst = blocks[k]
            sl = k % 2
            fv = stg[sl].t[:, 0:KC * cb].rearrange("p (kc n) -> p kc n", kc=KC)
            bv = stb[sl].t[:, 0:KC * cb].rearrange("p (kc n) -> p kc n", kc=KC)
            for kc in range(KC):
                sc = None if scale is None else scale[:, kc:kc + 1]
                self.cv(bv[:, kc, :], fv[:, kc, :], sc, r=[stg[sl]] + ([scale_res] if scale is not None else []), w=[stb[sl]])
            self.dma(dst, bv, r=[stb[sl]])

        for l in range(self.nlayer):
            self.dma(nw.t[:, l, 0, :], d["norm_mix_w"][l].rearrange("(kc p) -> p kc", p=128), w=[nw], slow=True)
            self.dma(nw.t[:, l, 1, 0:4], d["nsa_norm_w"][l].rearrange("(kc p) -> p kc", p=128), w=[nw], slow=True)
            self.dma(nw.t[:, l, 1, 4:8], d["ssd_norm_w"][l].rearrange("(kc p) -> p kc", p=128), w=[nw], slow=True)
            self.dma(nw.t[:, l, 2, :], d["norm_ffn_w"][l].rearrange("(kc p) -> p kc", p=128), w=[nw], slow=True)
        for l in range(self.nlayer):
            prep(d["w_in"][l], 8, INC, WIN_SEGS, d["win_s"][l], nw.t[:, l, 0, :], nw)
            prep(d["w_out"][l], 8, D, [(0, D)], d["wout_s"][l], nw.t[:, l, 1, :], nw)
            prep(d["w_gate"][l], 8, DFF, [(0, DFF)], d["wg_s"][l], nw.t[:, l, 2, :], nw)
            prep(d["w_up"][l], 8, DFF, [(0, DFF)], d["wu_s"][l], nw.t[:, l, 2, :], nw)
            prep(d["w_down"][l], NFF, D, [(0, D)], d["wd_s"][l], None, None)
        emit_load(0)
        for k in range(len(blocks)):
            if k + 1 < len(blocks):
                emit_load(k + 1)
            emit_conv(k)
        self.blk = len(blocks)
        for l in range(self.nlayer):
            for kv, (w1n, posn, b1n, w2n) in enumerate((("cmp_w1_k", "cmp_pos_k", "cmp_b1_k", "cmp_w2_k"),
                                                        ("cmp_w1_v", "cmp_pos_v", "cmp_b1_v", "cmp_w2_v"))):
                sl = self.blk % 2
                self.blk += 1
                fv = stg[sl].t[0:64, 0:4096].rearrange("p (l n) -> p l n", l=32)
                bv = stb[sl].t[0:64, 0:4096].rearrange("p (l n) -> p l n", l=32)
                self.dma(fv, d[w1n][l].rearrange("(l dd) n -> dd l n", dd=64), w=[stg[sl]])
                post = stg[sl].t[0:64, 4096:4128]
                self.dma(post, d[posn][l].rearrange("l dd -> dd l"), w=[stg[sl]], slow=True)
                b1t = stg[sl].t[:, 4200:4201]
                self.dma(b1t, d[b1n][l].rearrange("(n o) -> n o", o=1), w=[stg[sl]])
                w2t = stg[sl].t[:, 4300:4364]
                self.dma(w2t, d[w2n][l], w=[stg[sl]])
                self.p.dve(lambda e, o_=bv, i_=fv: e.tensor_copy(o_, i_), r=[stg[sl]], w=[stb[sl]])
                for hf in range(2):
                    self.dma(d["w1_s"][l, kv, hf * 64:(hf + 1) * 64], bv, r=[stb[sl]])
                ps = self.psf[0]
                for li in range(32):
                    self.mm(ps.t[:, 0:1], fv[:, li, :], post[:, li:li + 1], li == 0, li == 31, r=[stg[sl]], w=[ps])
                self.tt(self.cbias.t[:, l, kv:kv + 1], ps.t[:, 0:1], b1t, ALU.add, r=[ps, stg[sl]], w=[self.cbias])
                if kv == 0:
                    for g in range(2):
                        self.p.dve(lambda e, o_=self.w2k.t[:, l, g, g * 64:(g + 1) * 64], i_=w2t: e.tensor_copy(o_, i_),
                                   r=[stg[sl]], w=[self.w2k])
                else:
                    self.p.dve(lambda e, o_=self.w2v.t[:, l, :], i_=w2t: e.tensor_copy(o_, i_), r=[stg[sl]], w=[self.w2v])
        self.pro_res = stg + stb + [nw]

    def next_ps(self):
        self.psi = (getattr(self, "psi", -1) + 1) % len(self.psf)
        return self.psf[self.psi]

    def next_psb(self):
        self.psbi = (getattr(self, "psbi", -1) + 1) % len(self.psb)
        return self.psb[self.psbi]

    def load_w(self, src, n):
        self.wsi = (getattr(self, "wsi", -1) + 1) % len(self.wslot)
        ws = self.wslot[self.wsi]
        kc = src.shape[1]
        ap = ws.t[:, 0:kc * n].rearrange("p (k n) -> p k n", k=kc)
        self.dma(ap, src, r=[self.scr], w=[ws])
        return ws, ap

    def to_T(self, src_ap, n, dst_ap, r, w):
        pb = self.next_psb()
        for k in range(n):
            self.tr(pb.t[:, k * 128:(k + 1) * 128], src_ap[:, k * 128:(k + 1) * 128], self.identb.t[:, :], r=r + [self.identb], w=[pb])
        self.cp(dst_ap, pb.t[:, 0:n * 128].rearrange("p (k t) -> p k t", k=n), r=[pb], w=w)

    def rms_scale(self, ssq_ap, out_ap, n, r, w):
        self.ts(out_ap, ssq_ap, 1.0 / n, 1e-6, ALU.mult, ALU.add, r=r, w=w)
        self.actf(out_ap, out_ap, AF.Sqrt, r=w, w=w)
        self.p.dve(lambda e: e.reciprocal(out_ap, out_ap), r=w, w=w)

    def norm_T(self, xsrc):
        for tt in range(NT):
            xt = self.xtile[tt % 2]
            self.dma(xt.t[:, :], xsrc(tt), r=self.xsr[tt], w=[xt], q=XQ)
            st = self.nst[tt % 2]
            self.p.dve(lambda e, o_=st.t[:, 0:1]: e.memset(o_, 0.0), w=[st])
            self.actf(self.junkb.t[:, :], xt.t[:, :], AF.Square, r=[xt, st], w=[self.junkb, st], accum=st.t[:, 0:1])
            self.rms_scale(st.t[:, 0:1], st.t[:, 1:2], D, r=[st], w=[st])
            xn = self.xn[tt % 2]
            self.ts(xn.t[:, :], xt.t[:, :], st.t[:, 1:2], None, ALU.mult, r=[xt, st], w=[xn])
            self.to_T(xn.t, 8, self.xT.t[:, :, tt * 128:(tt + 1) * 128], r=[xn], w=[self.xTr[tt]])

    def resid_load(self, tt, nh, xsrc):
        self.xhi = (getattr(self, "xhi", -1) + 1) % len(self.xh)
        xh = self.xh[self.xhi]
        self.dma(xh.t[:, :], xsrc(tt)[:, nh * 512:(nh + 1) * 512], r=[self.xsr[tt][nh]], w=[xh], q=XQ)
        return xh

    def resid_fin(self, tt, nh, ps, xh, xdst, lag=2):
        self.tt(xh.t[:, :], xh.t[:, :], ps.t[:, :], ALU.add, r=[ps, xh], w=[xh])
        if not hasattr(self, "pst"):
            self.pst = []
        self.pst.append((xdst(tt)[:, nh * 512:(nh + 1) * 512], xh, self.xsr[tt][nh]))
        self.flush_stores(lag)

    def flush_stores(self, keep=0):
        pst = getattr(self, "pst", [])
        while len(pst) > keep:
            dst, xh, xr = pst.pop(0)
            self.dma(dst, xh.t[:, :], r=[xh], w=[xr], q="sp")

    def resid_add(self, tt, nh, ps, xsrc, xdst):
        xh = self.resid_load(tt, nh, xsrc)
        self.resid_fin(tt, nh, ps, xh, xdst)

    def ssd_phase(self, l, xsrc, xdst):
        p, d = self.p, self.d
        m0 = self.ar.off
        xs_tok = self.sb("xs_tok", [128, NT, 512], BF16)
        B_tok = self.sb("B_tok", [128, NT, 256], BF16)
        BT = self.sb("BT", [128, 2, S], BF16)
        CT = self.sb("CT", [128, 2, S], BF16)
        Wzdt = self.sb("Wzdt", [128, 8, 520], BF16)
        Wos = self.sb("Wos", [128, 4, D], BF16)
        zs = [[self.sb("zs%d_%d" % (i, u), [128, 512], BF16) for u in range(2)] for i in range(2)]
        ysb = [[self.sb("ysb%d_%d" % (i, u), [128, 512], BF16) for u in range(2)] for i in range(2)]
        ytmp = [self.sb("ytmp%d" % u, [128, 512], F32) for u in range(2)]
        dtt = [[self.sb("dtt%d_%d" % (i, u), [128, 40], F32) for u in range(2)] for i in range(2)]
        xdt = [[self.sb("xdt%d_%d" % (i, u), [128, 8, 64], BF16) for u in range(2)] for i in range(2)]
        xdd = [[self.sb("xdd%d_%d" % (i, u), [128, 8, 64], BF16) for u in range(2)] for i in range(2)]
        EA = [self.sb("EA%d" % i, [128, 256], F32) for i in range(3)]
        sgm = [[self.sb("sgm%d_%d" % (i, v), [128, 256], F32) for v in range(2)] for i in range(3)]
        MT = [[self.sb("MT%d_%d" % (i, v), [128, 256], BF16) for v in range(2)] for i in range(3)]
        CeT = [self.sb("CeT%d" % i, [128, 256], BF16) for i in range(3)]
        Sst = self.sb("Sst", [128, 2, 256], F32)
        Sbf = self.sb("Sbf", [128, 2, 256], BF16)
        ynb = [self.sb("ynb%d" % u, [128, 512], BF16) for u in range(2)]
        yT = [self.sb("yT%d" % u, [128, 4, 128], BF16) for u in range(2)]
        yst = [self.sb("yst%d" % u, [128, 4], F32) for u in range(2)]
        triu = self.sb("triu", [128, 2, 256], F32)
        tri01 = self.sb("tri01", [128, 128], F32)
        ones = self.sb("ones", [128, 128], F32)
        m1 = self.ar.off
        ctmp = self.sb("ctmp", [128, 1024 + 3], F32)
        cacc = self.sb("cacc", [128, 1024], F32)
        fmT = self.sb("fmT", [128, S], BF16)
        self.ar.off = m1
        Arep = [self.sb("Arep%d" % u, [128, 8, 128], F32) for u in range(2)]
        cbm = [self.sb("cbm%d" % g, [128, 2, 256], F32) for g in range(2)]
        self.ar.off = max(self.ar.off, m1 + 4112 + 4096 + 4096 + 192)
        p.dve(lambda e: e.memset(ctmp.t[:, 0:3], 0.0), w=[ctmp])
        for ch in range(8):
            if ch % 4 == 0:
                ws, wap = self.load_w(d["win_s"][l][:, :, XBC0 + ch * 128:XBC0 + ch * 128 + 512], 512)
            if ch == 1:
                self.dma(triu.t[:, :, :], d["c_triu"], w=[triu])
                self.dma(tri01.t[:, :], d["c_tri01"], w=[tri01])
                self.dma(ones.t[:, :], d["c_ones"], w=[ones])
                self.dma(Wzdt.t[:, :, :], d["win_s"][l][:, :, DTR0:DTR0 + 520], r=[self.scr], w=[Wzdt])
                self.dma(Wos.t[:, :, :], d["wout_s"][l][:, 4:8, :], r=[self.scr], w=[Wos])
            if ch < 4:
                dst, dres = fmT.t[:, :], fmT
            elif ch < 6:
                dst, dres = BT.t[:, ch - 4, :], BT
            else:
                dst, dres = CT.t[:, ch - 6, :], CT
            cw = self.sconv.t[:, l, ch, :]
            for hf in range(2):
                if hf == 0:
                    p.dve(lambda e: e.memset(ctmp.t[:, 0:3], 0.0), w=[ctmp])
                else:
                    self.cp(ctmp.t[:, 0:3], ctmp.t[:, 1024:1027], r=[ctmp], w=[ctmp], eng="dve")
                for I2 in range(2):
                    I = hf * 2 + I2
                    ps = self.next_ps()
                    for kc in range(8):
                        self.mm(ps.t[:, :], wap[:, kc, (ch % 4) * 128:(ch % 4 + 1) * 128], self.xT.t[:, kc, I * 512:(I + 1) * 512],
                                kc == 0, kc == 7, r=[ws] + self.xTr[4 * I:4 * I + 4], w=[ps])
                    self.cp(ctmp.t[:, 3 + I2 * 512:3 + (I2 + 1) * 512], ps.t[:, :], r=[ps], w=[ctmp])
                self.actf(cacc.t[:, :], ctmp.t[:, 3:3 + 1024], AF.Identity, r=[ctmp, self.sconv, self.sconvb], w=[cacc],
                          scale=cw[:, 3:4], bias=self.sconvb.t[:, l, ch:ch + 1])
                for k in range(3):
                    self.stt(cacc.t[:, :], ctmp.t[:, k:k + 1024], cw[:, k:k + 1], cacc.t[:, :], ALU.mult, ALU.add,
                             r=[ctmp, cacc, self.sconv], w=[cacc], eng="dve")
                self.actf(dst[:, hf * 1024:(hf + 1) * 1024], cacc.t[:, :], AF.Silu, r=[cacc], w=[dres])
            if ch < 6:
                for h8 in range(2):
                    pb = self.next_psb()
                    for k in range(8):
                        tk = h8 * 8 + k
                        self.tr(pb.t[:, k * 128:(k + 1) * 128], dst[:, tk * 128:(tk + 1) * 128], self.identb.t[:, :],
                                r=[dres, self.identb], w=[pb])
                    if ch < 4:
                        o_ = xs_tok.t[:, h8 * 8:h8 * 8 + 8, ch * 128:(ch + 1) * 128]
                        ores = xs_tok
                    else:
                        o_ = B_tok.t[:, h8 * 8:h8 * 8 + 8, (ch - 4) * 128:(ch - 3) * 128]
                        ores = B_tok
                    self.cp(o_, pb.t[:, :].rearrange("p (k t) -> p k t", k=8), r=[pb], w=[ores])
        p.dve(lambda e: e.memset(self.junkb.t[:, 0:1], 0.0), r=[ctmp, cacc, fmT], w=[self.junkb] + Arep + cbm)
        p.dve(lambda e: e.memset(Sst.t[:, :, :], 0.0), w=[Sst])
        p.dve(lambda e: e.memset(Sbf.t[:, :, :], 0.0), w=[Sbf])
        dtb, Ab, Db = self.dtb.t[:, l * 8:(l + 1) * 8], self.Ab.t[:, l * 8:(l + 1) * 8], self.Db.t[:, l * 8:(l + 1) * 8]
        prm = [self.dtb, self.Ab, self.Db]
        small = self.psf[5]
        psy = [self.psf[3], self.psf[4]]

        def P1(c):
            for u in range(2):
                tt = 2 * c + u
                z_ = zs[c % 2][u]
                xcols = self.xT.t[:, :, tt * 128:(tt + 1) * 128]
                psz = self.next_ps5()
                for kc in range(8):
                    self.mm(psz.t[:, :], xcols[:, kc, :], Wzdt.t[:, kc, 8:520], kc == 0, kc == 7, r=[Wzdt, self.xTr[tt]], w=[psz])
                for kc in range(8):
                    self.mm(small.t[:, u * 8:u * 8 + 8], xcols[:, kc, :], Wzdt.t[:, kc, 0:8], kc == 0, kc == 7,
                            r=[Wzdt, self.xTr[tt]], w=[small])
                self.actf(z_.t[:, :], psz.t[:, :], AF.Silu, r=[psz], w=[z_])
                dq = dtt[c % 2][u]
                self.tt(dq.t[:, 0:8], small.t[:, u * 8:u * 8 + 8], dtb, ALU.add, r=[small] + prm, w=[dq])
                self.actf(dq.t[:, 0:8], dq.t[:, 0:8], AF.Exp, r=[dq], w=[dq])
                self.actf(dq.t[:, 0:8], dq.t[:, 0:8], AF.Ln, r=[dq], w=[dq], bias=1.0)
                self.tt(dq.t[:, 8:16], dq.t[:, 0:8], Ab, ALU.mult, r=[dq] + prm, w=[dq])

        def P1b(c):
            for u in range(2):
                tt = 2 * c + u
                dq = dtt[c % 2][u]
                self.tt(xdt[c % 2][u].t[:, :, :], xs_tok.t[:, tt, :].rearrange("p (h e) -> p h e", h=8),
                        dq.t[:, 0:8].unsqueeze(2).to_broadcast([128, 8, 64]), ALU.mult, r=[xs_tok, dq], w=[xdt[c % 2][u]])
                self.p.dve(lambda e, o_=Arep[u].t[:, :, :], i_=dq.t[:, 8:16].unsqueeze(2).to_broadcast([128, 8, 128]):
                           e.tensor_copy(o_, i_), r=[dq], w=[Arep[u]])
            for u in range(2):
                for kt in range(u + 1):
                    self.mm(small.t[:, 16 + u * 8:24 + u * 8], triu.t[:, kt, u * 128:(u + 1) * 128], dtt[c % 2][kt].t[:, 8:16],
                            kt == 0, kt == u, r=[triu, dtt[c % 2][kt]], w=[small])
            for kt in range(2):
                self.mm(small.t[:, 32:40], ones.t[:, :], dtt[c % 2][kt].t[:, 8:16], kt == 0, kt == 1, r=[ones, dtt[c % 2][kt]], w=[small])

        def P1c(c):
            for g in range(2):
                pcb = self.next_ps5()
                for v in range(2):
                    self.mm(pcb.t[:, v * 256:(v + 1) * 256], BT.t[:, g, (2 * c + v) * 128:(2 * c + v + 1) * 128],
                            CT.t[:, g, c * 256:(c + 1) * 256], True, True, r=[BT, CT], w=[pcb])
                self.cp(cbm[g].t[:, :, :], pcb.t[:, :].rearrange("p (v n) -> p v n", v=2), r=[pcb], w=[cbm[g]], eng="act")
                for v in range(2):
                    self.tt(cbm[g].t[:, v, v * 128:(v + 1) * 128], cbm[g].t[:, v, v * 128:(v + 1) * 128], tri01.t[:, :],
                            ALU.mult, r=[cbm[g], tri01], w=[cbm[g]])

        def P2(c):
            for u in range(2):
                dq = dtt[c % 2][u]
                self.cp(dq.t[:, 16:24], small.t[:, 16 + u * 8:24 + u * 8], r=[small], w=[dq], eng="dve")
                self.tt(dq.t[:, 24:32], small.t[:, 32:40], dq.t[:, 16:24], ALU.subtract, r=[small, dq], w=[dq])
                self.cp(dq.t[:, 32:40], small.t[:, 32:40], r=[small], w=[dq], eng="dve")
                self.actf(dq.t[:, 24:40], dq.t[:, 24:40], AF.Exp, r=[dq], w=[dq])
                self.tt(xdd[c % 2][u].t[:, :, :], xdt[c % 2][u].t[:, :, :], dq.t[:, 24:32].unsqueeze(2).to_broadcast([128, 8, 64]),
                        ALU.mult, r=[xdt[c % 2][u], dq], w=[xdd[c % 2][u]])

        def heads(c, hooks):
            prs = {}

            def S1(h):
                pr = self.next_ps5()
                prs[h] = pr
                for kt in range(2):
                    self.mm(pr.t[:, 0:256], Arep[kt].t[:, h, :], triu.t[:, kt, :], kt == 0, kt == 1, r=[Arep[kt], triu], w=[pr])

            def S2a(h):
                i3 = h % 3
                self.actf(EA[i3].t[:, :], prs[h].t[:, 0:256], AF.Exp, r=[prs[h]], w=[EA[i3]])

            def S2b(h):
                g, i3 = h // 4, h % 3
                pr = prs.pop(h)
                for v in range(2):
                    lo = v * 128
                    self.ts(sgm[i3][v].t[:, lo:256], pr.t[:, lo:256], dtt[c % 2][v].t[:, 16 + h:17 + h], 0.0, ALU.subtract, ALU.min,
                            r=[pr, dtt[c % 2][v], EA[i3]], w=[sgm[i3][v]])
                self.tt(CeT[i3].t[:, :], CT.t[:, g, c * 256:(c + 1) * 256], EA[i3].t[:, :], ALU.mult, r=[CT, EA[i3]], w=[CeT[i3]])

            def S2c(h):
                i3 = h % 3
                for v in range(2):
                    lo = v * 128
                    self.actf(sgm[i3][v].t[:, lo:256], sgm[i3][v].t[:, lo:256], AF.Exp, r=[sgm[i3][v]], w=[sgm[i3][v]])

            def S2d(h):
                g, i3 = h // 4, h % 3
                for v in range(2):
                    lo = v * 128
                    self.tt(MT[i3][v].t[:, lo:256], sgm[i3][v].t[:, lo:256], cbm[g].t[:, v, lo:256], ALU.mult,
                            r=[sgm[i3][v], cbm[g]], w=[MT[i3][v]])

            def S3(h):
                g, i3 = h // 4, h % 3
                for u in range(2):
                    o_ = psy[u].t[:, h * 64:(h + 1) * 64]
                    for v in range(u + 1):
                        self.mm(o_, MT[i3][v].t[:, u * 128:(u + 1) * 128], xdt[c % 2][v].t[:, h, :], v == 0, False,
                                r=[MT[i3][v], xdt[c % 2][v]], w=[psy[u]])
                    self.mm(o_, CeT[i3].t[:, u * 128:(u + 1) * 128], Sbf.t[:, g, (h % 4) * 64:(h % 4 + 1) * 64], False, True,
                            r=[CeT[i3], Sbf], w=[psy[u]])

            for t in range(8 + 3):
                if 1 <= t <= 8:
                    S2a(t - 1)
                    S2b(t - 1)
                for fn in hooks.get(t, []):
                    fn()
                if t < 8:
                    S1(t)
                if 2 <= t <= 9:
                    S2c(t - 2)
                    S2d(t - 2)
                if t >= 3:
                    S3(t - 3)

        def E(c):
            for u in range(2):
                self.cp(ysb[c % 2][u].t[:, :], psy[u].t[:, :], r=[psy[u]], w=[ysb[c % 2][u]], eng="act")
            for g in range(2):
                pss = self.next_ps5()
                for u in range(2):
                    self.mm(pss.t[:, 0:256], B_tok.t[:, 2 * c + u, g * 128:(g + 1) * 128],
                            xdd[c % 2][u].t[:, 4 * g:4 * g + 4, :].rearrange("p h e -> p (h e)"), u == 0, u == 1,
                            r=[B_tok, xdd[c % 2][u]], w=[pss])
                for r4 in range(4):
                    h = 4 * g + r4
                    self.stt(Sst.t[:, g, r4 * 64:(r4 + 1) * 64], Sst.t[:, g, r4 * 64:(r4 + 1) * 64], dtt[c % 2][0].t[:, 32 + h:33 + h],
                             pss.t[:, r4 * 64:(r4 + 1) * 64], ALU.mult, ALU.add, r=[Sst, dtt[c % 2][0], pss], w=[Sst])
                self.cp(Sbf.t[:, g, :], Sst.t[:, g, :], r=[Sst], w=[Sbf], eng="dve")

        def F(c, u):
            tt = 2 * c + u
            y_ = ytmp[u]
            z_ = zs[c % 2][u]
            self.tt(y_.t[:, :].rearrange("p (h e) -> p h e", h=8), xs_tok.t[:, tt, :].rearrange("p (h e) -> p h e", h=8),
                    Db.unsqueeze(2).to_broadcast([128, 8, 64]), ALU.mult, r=[xs_tok] + prm, w=[y_])
            self.tt(y_.t[:, :], y_.t[:, :], ysb[c % 2][u].t[:, :], ALU.add, r=[y_, ysb[c % 2][u]], w=[y_])
            self.tt(y_.t[:, :], y_.t[:, :], z_.t[:, :], ALU.mult, r=[y_, z_], w=[y_])
            self.p.dve(lambda e, o_=yst[u].t[:, 0:2]: e.memset(o_, 0.0), w=[yst[u]])
            for g in range(2):
                self.actf(self.junkb.t[:, 0:256], y_.t[:, g * 256:(g + 1) * 256], AF.Square, r=[y_, yst[u]],
                          w=[self.junkb, yst[u]], accum=yst[u].t[:, g:g + 1])
            self.rms_scale(yst[u].t[:, 0:2], yst[u].t[:, 2:4], 256, r=[yst[u]], w=[yst[u]])
            for g in range(2):
                self.ts(ynb[u].t[:, g * 256:(g + 1) * 256], y_.t[:, g * 256:(g + 1) * 256], yst[u].t[:, 2 + g:3 + g], None,
                        ALU.mult, r=[y_, yst[u]], w=[ynb[u]])
            self.to_T(ynb[u].t, 4, yT[u].t[:, :, :], r=[ynb[u]], w=[yT[u]])
            for nh in range(2):
                po = self.next_ps5()
                xh = self.resid_load(tt, nh, xsrc)
                for k4 in range(4):
                    self.mm(po.t[:, :], yT[u].t[:, k4, :], Wos.t[:, k4, nh * 512:(nh + 1) * 512], k4 == 0, k4 == 3,
                            r=[yT[u], Wos], w=[po])
                self.resid_fin(tt, nh, po, xh, xdst)

        P1(0)
        P1b(0)
        P1c(0)
        P2(0)
        for c in range(8):
            hooks = {}
            if c >= 1:
                hooks.setdefault(1, []).append(lambda c=c: F(c - 1, 0))
                hooks.setdefault(4, []).append(lambda c=c: F(c - 1, 1))
            if c < 7:
                hooks.setdefault(6, []).append(lambda c=c: P1(c + 1))
                hooks.setdefault(8, []).append(lambda c=c: P1b(c + 1))
                hooks.setdefault(10, []).append(lambda c=c: (P1c(c + 1), P2(c + 1)))
            heads(c, hooks)
            E(c)
        F(7, 0)
        F(7, 1)
        self.flush_stores(0)
        self.ar.off = m0

    def next_ps5(self):
        self.ps5i = (getattr(self, "ps5i", -1) + 1) % 3
        return self.psf[self.ps5i]

    def nsa_phase(self, l, xsrc, xdst):
        p, d = self.p, self.d
        m0 = self.ar.off
        qT = self.sb("qT", [128, 4, S], BF16)
        kT = [self.sb("kT%d" % i, [128, S], BF16) for i in range(4)]
        vsa = self.sb("vsa", [128, NT, 2, 65], BF16)
        vwa = self.sb("vwa", [128, NT, 2, 65], BF16)
        gates = self.sb("gates", [128, NT, 24], F32)
        w1 = [self.sb("w1_%d" % i, [128, 32, 128], BF16) for i in range(2)]
        causal = self.sb("causal", [128, 4, 512], BF16)
        band = self.sb("band", [128, 4, 512], BF16)
        cmpb = self.sb("cmpb", [128, S], BF16)
        eexp = self.sb("eexp", [32, NT, 128], BF16)
        fsel = self.sb("fsel", [128, 8, 32], F32)
        forc = self.sb("forc", [128, 8, 32], F32)
        Woa = self.sb("Woa", [128, 4, D], BF16)
        kcmpT = self.sb("kcmpT", [128, 128], BF16)
        rhsc = self.sb("rhsc", [128, 2, 97], F32)
        hx = [self.sb("hx%d" % i, [128, 128], F32) for i in range(3)]
        hidb = [self.sb("hidb%d" % g, [128, 128], BF16) for g in range(2)]
        selbT = self.sb("selbT", [32, 2, 1024], BF16)
        ocur = self.sb("ocur", [128, 4, 512], F32)
        PT = [self.sb("PT%d" % i, [128, 512], BF16) for i in range(4)]
        PcT = [self.sb("PcT%d" % i, [128, 512], F32) for i in range(2)]
        impacc = [self.sb("impacc%d" % g, [128, 4, 32], F32) for g in range(2)]
        imptmp = self.sb("imptmp", [128, 4, 32], F32)
        sm = [self.sb("sm%d" % i, [128, 16], F32) for i in range(2)]
        sc = self.sb("sc", [128, 32], F32)
        wk = self.sb("wk", [128, 32], F32)
        m8 = self.sb("m8", [128, 16], F32)
        selb = self.sb("selb", [128, 32], BF16)
        onb = [self.sb("onb%d" % i, [128, 512], BF16) for i in range(2)]
        ost = [self.sb("ost%d" % i, [128, 2], F32) for i in range(2)]
        p.dve(lambda e: e.memset(vsa.t[:, :, :, 64:65], 1.0), w=[vsa])
        p.dve(lambda e: e.memset(vwa.t[:, :, :, 64:65], 1.0), w=[vwa])
        p.dve(lambda e: e.memset(kcmpT.t[:, :], 0.0), w=[kcmpT])
        p.dve(lambda e: e.memset(rhsc.t[:, :, :], 0.0), w=[rhsc])
        for g in range(2):
            p.dve(lambda e, g=g: e.memset(hidb[g].t[:, :], 0.0), w=[hidb[g]])
            p.dve(lambda e, g=g: e.tensor_copy(rhsc.t[:, g, 0:32], self.ovl.t[:, :]), r=[self.ovl], w=[rhsc])
            p.dve(lambda e, g=g: e.memset(rhsc.t[:, g, 96:97], 1.0), w=[rhsc])
        fm_dst = [(qT, qT.t[:, 0, :]), (qT, qT.t[:, 1, :]), (qT, qT.t[:, 2, :]), (qT, qT.t[:, 3, :]),
                  (kT[0], kT[0].t[:, :]), (kT[1], kT[1].t[:, :]), (kT[2], kT[2].t[:, :]), (kT[3], kT[3].t[:, :])]
        for ch in range(8):
            if ch % 4 == 0:
                ws, wap = self.load_w(d["win_s"][l][:, :, ch * 128:ch * 128 + 512], 512)
            for I in range(4):
                ps = self.next_ps()
                for kc in range(8):
                    self.mm(ps.t[:, :], wap[:, kc, (ch % 4) * 128:(ch % 4 + 1) * 128], self.xT.t[:, kc, I * 512:(I + 1) * 512],
                            kc == 0, kc == 7, r=[ws] + self.xTr[4 * I:4 * I + 4], w=[ps])
                self.cp(fm_dst[ch][1][:, I * 512:(I + 1) * 512], ps.t[:, :], r=[ps], w=[fm_dst[ch][0]])
        ws, wap = self.load_w(d["win_s"][l][:, :, VS0:VS0 + 280], 280)
        for kv in range(2):
            self.dma(w1[kv].t[:, :, :], d["w1_s"][l, kv], r=[self.scr], w=[w1[kv]])
        for (t_, nm) in ((cmpb, "c_cmpb"), (causal, "c_causal"), (band, "c_band"), (eexp, "c_eexp"), (fsel, "c_fsel"), (forc, "c_forc")):
            self.dma(t_.t[:], d[nm], w=[t_])
        self.dma(Woa.t[:, :, :], d["wout_s"][l][:, 0:4, :], r=[self.scr], w=[Woa])
        for tt in range(NT):
            ps = self.next_ps()
            for kc in range(8):
                self.mm(ps.t[:, 0:280], self.xT.t[:, kc, tt * 128:(tt + 1) * 128], wap[:, kc, :], kc == 0, kc == 7,
                        r=[ws, self.xTr[tt]], w=[ps])
            self.cp(vsa.t[:, tt, :, 0:64], ps.t[:, 0:128].rearrange("p (g e) -> p g e", g=2), r=[ps], w=[vsa], eng="act")
            self.cp(vwa.t[:, tt, :, 0:64], ps.t[:, 128:256].rearrange("p (g e) -> p g e", g=2), r=[ps], w=[vwa], eng="act")
            self.actf(gates.t[:, tt, :], ps.t[:, 256:280], AF.Sigmoid, r=[ps], w=[gates])
        for kv in range(2):
            for g in range(2):
                ps = self.next_ps()
                for li in range(32):
                    self.mm(ps.t[:, 0:127], w1[kv].t[g * 64:(g + 1) * 64, li, :], kT[kv].t[g * 64:(g + 1) * 64, li:li + 16 * 126 + 1:16],
                            li == 0, li == 31, r=[w1[kv], kT[kv]], w=[ps])
                xg, x2, sg = hx[0], hx[1], hx[2]
                self.actf(xg.t[:, 0:127], ps.t[:, 0:127], AF.Identity, r=[ps, self.cbias], w=[xg], bias=self.cbias.t[:, l, kv:kv + 1])
                self.tt(x2.t[:, 0:127], xg.t[:, 0:127], xg.t[:, 0:127], ALU.mult, r=[xg], w=[x2])
                self.ts(x2.t[:, 0:127], x2.t[:, 0:127], 0.044715, 1.0, ALU.mult, ALU.add, r=[x2], w=[x2])
                self.tt(x2.t[:, 0:127], x2.t[:, 0:127], xg.t[:, 0:127], ALU.mult, r=[x2, xg], w=[x2])
                self.actf(sg.t[:, 0:127], x2.t[:, 0:127], AF.Sigmoid, r=[x2], w=[sg], scale=1.5957691216057308)
                self.tt(hidb[g].t[:, 0:127], xg.t[:, 0:127], sg.t[:, 0:127], ALU.mult, r=[xg, sg], w=[hidb[g]])
                if kv == 1:
                    pv = self.next_ps()
                    self.mm(pv.t[0:127, 0:64], hidb[g].t[:, 0:127], self.w2v.t[:, l, :], True, True, r=[hidb[g], self.w2v], w=[pv])
                    self.cp(rhsc.t[0:127, g, 32:96], pv.t[0:127, 0:64], r=[pv], w=[rhsc], eng="dve")
            if kv == 0:
                pk = self.next_ps()
                for g in range(2):
                    self.mm(pk.t[:, 0:127], self.w2k.t[:, l, g, :], hidb[g].t[:, 0:127], g == 0, g == 1, r=[self.w2k, hidb[g]], w=[pk])
                self.cp(kcmpT.t[:, 0:127], pk.t[:, 0:127], r=[pk], w=[kcmpT], eng="dve")
        ksT, kwT = kT[2], kT[3]
        pcb = [self.psf[3], self.psf[4], self.psf[5]]
        SK = 2
        hh = 0
        for I in range(4):
            tiles = slice(4 * I, 4 * I + 4)

            def cmp_score(h):
                g, r4 = h // 4, h % 4
                gp = slice(g * 64, (g + 1) * 64)
                q = qT.t[gp, r4, I * 512:(I + 1) * 512]
                ps = self.next_ps5()
                self.mm(ps.t[:, :], kcmpT.t[gp, :], q, True, False, r=[kcmpT, qT], w=[ps])
                self.mm(ps.t[:, :], self.identb.t[:, :], cmpb.t[:, I * 512:(I + 1) * 512], False, True, r=[self.identb, cmpb], w=[ps])
                self.actf(PcT[h % 2].t[:, :], ps.t[:, :], AF.Exp, r=[ps], w=[PcT[h % 2]], scale=0.125)

            def cmp_pv(h):
                g, r4 = h // 4, h % 4
                pc_ = PcT[h % 2]
                pc = pcb[h % 3]
                for sub in range(4):
                    self.mm(pc.t[:, sub * 97:(sub + 1) * 97], pc_.t[:, sub * 128:(sub + 1) * 128], rhsc.t[:, g, :], True, True,
                            r=[pc_, rhsc], w=[pc])
                pcv = pc.t[:, 0:388].rearrange("p (s n) -> p s n", s=4)
                s_ = sm[h % 2]
                self.ts(s_.t[:, 0:4], pcv[:, :, 96], 1e-30, None, ALU.max, r=[pc], w=[s_])
                self.p.dve(lambda e, o_=s_.t[:, 4:8], i_=s_.t[:, 0:4]: e.reciprocal(o_, i_), r=[s_], w=[s_])
                self.tt(s_.t[:, 8:12], s_.t[:, 4:8], gates.t[:, tiles, h * 3 + 0], ALU.mult, r=[s_, gates], w=[s_])
                rb = s_.t[:, 4:8].unsqueeze(2).to_broadcast([128, 4, 32])
                if r4 == 0:
                    self.tt(impacc[g].t[:, :, :], pcv[:, :, 0:32], rb, ALU.mult, r=[pc, s_], w=[impacc[g]])
                else:
                    self.tt(imptmp.t[:, :, :], pcv[:, :, 0:32], rb, ALU.mult, r=[pc, s_], w=[imptmp])
                    self.tt(impacc[g].t[:, :, :], impacc[g].t[:, :, :], imptmp.t[:, :, :], ALU.add, r=[imptmp, impacc[g]], w=[impacc[g]])
                for sub in range(4):
                    self.ts(ocur.t[:, sub, h * 64:(h + 1) * 64], pcv[:, sub, 32:96], s_.t[:, 8 + sub:9 + sub], None, ALU.mult,
                            r=[pc, s_], w=[ocur])

            for h in range(9):
                if h < 8:
                    cmp_score(h)
                if h >= 1:
                    cmp_pv(h - 1)
            if I >= 2:
                for g in range(2):
                    for sub in range(4):
                        ti = 4 * (I - 2) + sub
                        self.tt(sc.t[:, :], impacc[g].t[:, sub, :], fsel.t[:, ti, :], ALU.add, r=[impacc[g], fsel], w=[sc])
                        self.p.dve(lambda e: e.max(out=m8.t[:, 0:8], in_=sc.t[:, :]), r=[sc], w=[m8])
                        self.p.dve(lambda e: e.match_replace(out=wk.t[:, :], in_to_replace=m8.t[:, 0:8], in_values=sc.t[:, :], imm_value=-3e38),
                                   r=[sc, m8], w=[wk])
                        self.p.dve(lambda e: e.max(out=m8.t[:, 8:16], in_=wk.t[:, :]), r=[wk], w=[m8])
                        self.ts(wk.t[:, :], sc.t[:, :], m8.t[:, 12:13], None, ALU.is_ge, r=[sc, m8], w=[wk])
                        self.ts(wk.t[:, :], wk.t[:, :], -NEGB, None, ALU.mult, r=[wk], w=[wk])
                        self.tt(wk.t[:, :], wk.t[:, :], forc.t[:, ti, :], ALU.add, r=[wk, forc], w=[wk])
                        self.ts(selb.t[:, :], wk.t[:, :], NEGB, 0.0, ALU.add, ALU.min, r=[wk], w=[selb])
                        pb = self.next_psb()
                        self.tr(pb.t[0:32, 0:128], selb.t[:, :], self.identb.t[:, :], r=[selb, self.identb], w=[pb])
                        self.cp(selbT.t[:, g, ti * 128:(ti + 1) * 128], pb.t[0:32, 0:128], r=[pb], w=[selbT], eng="act")
            tl = []
            for br in (1, 0):
                for h in range(8):
                    j0 = 0 if br == 0 else max(0, 4 * I - 4)
                    for j in range(j0, 4 * I + 4):
                        tl.append((br, h, j, j == j0, j == 4 * I + 3, hh))
                    hh += 1
            pts = {}

            def score(ix):
                br, h, j, first, last, hn = tl[ix]
                kk = ksT if br == 0 else kwT
                g, r4 = h // 4, h % 4
                gp = slice(g * 64, (g + 1) * 64)
                mrel = 4 * I - j
                c_lo, c_hi = 0, 512
                extra = []
                if br == 0 and I >= 2:
                    extra.append(("sel", None))
                if mrel <= 0:
                    c_lo = 128 * (-mrel)
                    extra.append(("causal", -mrel))
                elif br == 1:
                    c_hi = 128 * (5 - mrel)
                    extra.append(("band", mrel - 1))
                ps = self.next_ps5()
                q = qT.t[gp, r4, I * 512 + c_lo:I * 512 + c_hi]
                self.mm(ps.t[:, c_lo:c_hi], kk.t[gp, j * 128:(j + 1) * 128], q, True, len(extra) == 0, r=[kk, qT], w=[ps])
                for ei, (kind, idx) in enumerate(extra):
                    lst = ei == len(extra) - 1
                    if kind == "sel":
                        self.mm(ps.t[:, c_lo:c_hi], eexp.t[:, j, :], selbT.t[:, g, (I - 2) * 512 + c_lo:(I - 2) * 512 + c_hi],
                                False, lst, r=[eexp, selbT], w=[ps])
                    elif kind == "causal":
                        self.mm(ps.t[:, c_lo:c_hi], self.identb.t[:, :], causal.t[:, idx, c_lo:c_hi], False, lst,
                                r=[self.identb, causal], w=[ps])
                    else:
                        self.mm(ps.t[:, c_lo:c_hi], self.identb.t[:, :], band.t[:, idx, c_lo:c_hi], False, lst,
                                r=[self.identb, band], w=[ps])
                pt = PT[ix % len(PT)]
                self.actf(pt.t[:, c_lo:c_hi], ps.t[:, c_lo:c_hi], AF.Exp, r=[ps], w=[pt], scale=0.125)
                pts[ix] = (pt, c_lo, c_hi)

            def pv(ix):
                br, h, j, first, last, hn = tl[ix]
                va = vsa if br == 0 else vwa
                g = h // 4
                po = self.psf[3 + hn % 2]
                pt, c_lo, c_hi = pts.pop(ix)
                if first:
                    self.mm(po.t[:, 0:260], self.zl.t[:, :], self.zr.t[:, :], True, False, r=[self.zl, self.zr], w=[po])
                for sub in range(c_lo // 128, c_hi // 128):
                    self.mm(po.t[:, sub * 65:(sub + 1) * 65], pt.t[:, sub * 128:(sub + 1) * 128], va.t[:, j, g, :],
                            False, j == 4 * I + sub, r=[pt, va], w=[po])
                if last:
                    pov = po.t[:, 0:260].rearrange("p (s n) -> p s n", s=4)
                    s_ = sm[hn % 2]
                    self.p.dve(lambda e, o_=s_.t[:, 4:8], i_=pov[:, :, 64]: e.reciprocal(o_, i_), r=[po], w=[s_])
                    self.tt(s_.t[:, 8:12], s_.t[:, 4:8], gates.t[:, tiles, h * 3 + 1 + br], ALU.mult, r=[s_, gates], w=[s_])
                    for sub in range(4):
                        oc = ocur.t[:, sub, h * 64:(h + 1) * 64]
                        self.stt(oc, pov[:, sub, 0:64], s_.t[:, 8 + sub:9 + sub], oc, ALU.mult, ALU.add, r=[po, s_, ocur], w=[ocur])

            for ix in range(len(tl) + SK):
                if ix < len(tl):
                    score(ix)
                if ix >= SK:
                    pv(ix - SK)
            for sub in range(4):
                tt = 4 * I + sub
                st = ost[sub % 2]
                self.p.dve(lambda e, o_=st.t[:, 0:1]: e.memset(o_, 0.0), w=[st])
                self.actf(self.junkb.t[:, 0:512], ocur.t[:, sub, :], AF.Square, r=[ocur, st], w=[self.junkb, st], accum=st.t[:, 0:1])
                self.rms_scale(st.t[:, 0:1], st.t[:, 1:2], 512, r=[st], w=[st])
                ob = onb[sub % 2]
                self.ts(ob.t[:, :], ocur.t[:, sub, :], st.t[:, 1:2], None, ALU.mult, r=[ocur, st], w=[ob])
                self.to_T(ob.t, 4, self.xT.t[:, 0:4, tt * 128:(tt + 1) * 128], r=[ob], w=[self.xTr[tt]])
        for tt in range(NT):
            for nh in range(2):
                po = self.next_ps()
                xh = self.resid_load(tt, nh, xsrc)
                for k4 in range(4):
                    self.mm(po.t[:, :], self.xT.t[:, k4, tt * 128:(tt + 1) * 128], Woa.t[:, k4, nh * 512:(nh + 1) * 512], k4 == 0, k4 == 3,
                            r=[self.xTr[tt], Woa], w=[po])
                self.resid_fin(tt, nh, po, xh, xdst)
        self.flush_stores(0)
        self.ar.off = m0

    def ffn_phase(self, l, xsrc, xdst):
        p, d = self.p, self.d
        m0 = self.ar.off
        act = self.sb("act", [128, NFF, 1024], BF16)
        gbuf = [self.sb("gbuf%d" % i, [128, 1026], F32) for i in range(2)]
        halo = self.sb("halo", [128, NFF, 2], F32)
        cacc = [self.sb("facc%d" % i, [128, 1024], F32) for i in range(2)]
        sact = [self.sb("sact%d" % i, [128, 1024], F32) for i in range(2)]
        NS, DA = 6, 4
        fsl = [self.sb("fsl%d" % i, [128, 4096], BF16) for i in range(NS)]
        blocks = []
        for half in range(2):
            for f0 in range(0, NFF, 4):
                n = min(512, DFF - f0 * 128)
                blocks.append((d["wg_s"][l][:, :, f0 * 128:f0 * 128 + n], 8, n))
                blocks.append((d["wu_s"][l][:, :, f0 * 128:f0 * 128 + n], 8, n))
            for tg in range(2):
                for nh in range(2):
                    for f0 in (0, 8, 16):
                        nf = min(8, NFF - f0)
                        blocks.append((d["wd_s"][l][:, f0:f0 + nf, nh * 512:(nh + 1) * 512], nf, 512))
        state = {"emitted": 0, "k": 0}

        def get():
            k = state["k"]
            state["k"] += 1
            while state["emitted"] < min(len(blocks), k + DA + 1):
                e = state["emitted"]
                src, kc, n = blocks[e]
                ws = fsl[e % NS]
                self.dma(ws.t[:, 0:kc * n].rearrange("p (k n) -> p k n", k=kc), src, r=[self.scr], w=[ws])
                state["emitted"] += 1
            src, kc, n = blocks[k]
            ws = fsl[k % NS]
            return ws, ws.t[:, 0:kc * n].rearrange("p (k n) -> p k n", k=kc)

        for half in range(2):
            t0 = half * 1024
            for f in range(NFF):
                if f % 4 == 0:
                    wsg, wg = get()
                    wsu, wu = get()
                fc = slice((f % 4) * 128, (f % 4 + 1) * 128)
                gb, ca, sa = gbuf[f % 2], cacc[f % 2], sact[f % 2]
                if half == 0:
                    p.dve(lambda e, o_=gb.t[:, 0:2]: e.memset(o_, 0.0), w=[gb])
                else:
                    self.cp(gb.t[:, 0:2], halo.t[:, f, :], r=[halo], w=[gb], eng="dve")
                for I2 in range(2):
                    ps = self.next_ps()
                    for kc in range(8):
                        self.mm(ps.t[:, :], wg[:, kc, fc], self.xT.t[:, kc, t0 + I2 * 512:t0 + (I2 + 1) * 512], kc == 0, kc == 7,
                                r=[wsg] + self.xTr[(t0 // 128) + 4 * I2:(t0 // 128) + 4 * I2 + 4], w=[ps])
                    self.cp(gb.t[:, 2 + I2 * 512:2 + (I2 + 1) * 512], ps.t[:, :], r=[ps], w=[gb])
                if half == 0:
                    self.cp(halo.t[:, f, :], gb.t[:, 1024:1026], r=[gb], w=[halo], eng="dve")
                cw = self.fconv.t[:, l, f, :]
                self.actf(ca.t[:, :], gb.t[:, 2:1026], AF.Identity, r=[gb, self.fconv, self.fconvb], w=[ca],
                          scale=cw[:, 2:3], bias=self.fconvb.t[:, l, f:f + 1])
                self.stt(ca.t[:, :], gb.t[:, 1:1025], cw[:, 1:2], ca.t[:, :], ALU.mult, ALU.add, r=[gb, ca, self.fconv], w=[ca], eng="dve")
                self.stt(ca.t[:, :], gb.t[:, 0:1024], cw[:, 0:1], ca.t[:, :], ALU.mult, ALU.add, r=[gb, ca, self.fconv], w=[ca], eng="dve")
                self.actf(sa.t[:, :], ca.t[:, :], AF.Silu, r=[ca], w=[sa])
                for I2 in range(2):
                    ps = self.next_ps()
                    for kc in range(8):
                        self.mm(ps.t[:, :], wu[:, kc, fc], self.xT.t[:, kc, t0 + I2 * 512:t0 + (I2 + 1) * 512], kc == 0, kc == 7,
                                r=[wsu] + self.xTr[(t0 // 128) + 4 * I2:(t0 // 128) + 4 * I2 + 4], w=[ps])
                    self.tt(act.t[:, f, I2 * 512:(I2 + 1) * 512], sa.t[:, I2 * 512:(I2 + 1) * 512], ps.t[:, :], ALU.mult,
                            r=[sa, ps], w=[act])
            for tg in range(2):
                for nh in range(2):
                    accs = [self.next_ps() for _ in range(4)]
                    self.flush_stores(0)
                    xhs = [self.resid_load(half * 8 + tg * 4 + ti, nh, xsrc) for ti in range(4)]
                    for f0 in (0, 8, 16):
                        nf = min(8, NFF - f0)
                        wsd, wd = get()
                        for ti in range(4):
                            tl = tg * 4 + ti
                            for fi in range(nf):
                                f = f0 + fi
                                self.mm(accs[ti].t[:, :], act.t[:, f, tl * 128:(tl + 1) * 128], wd[:, fi, :], f == 0, f == NFF - 1,
                                        r=[act, wsd], w=[accs[ti]])
                    for ti in range(4):
                        self.resid_fin(half * 8 + tg * 4 + ti, nh, accs[ti], xhs[ti], xdst, lag=4)
        self.flush_stores(0)
        self.ar.off = m0

    def build(self):
        p, d, nc = self.p, self.d, self.nc
        self.psf = [Res(p.stack.enter_context(nc.psum_tensor("psf%d" % i, [128, 512], F32)), "psf%d" % i) for i in range(6)]
        self.psb = [Res(p.stack.enter_context(nc.psum_tensor("psb%d" % i, [128, 1024], BF16)), "psb%d" % i) for i in range(2)]
        self.scr = Res(None, "scr")
        self.xsr = [[Res(None, "xsr%d_%d" % (i, j)) for j in range(2)] for i in range(NT)]
        self.identb = self.sb("identb", [128, 128], BF16)
        self.ovl = self.sb("ovl", [128, 32], F32)
        self.nfw = self.sb("nfw", [128, D], F32)
        self.sconv = self.sb("sconv", [128, L, 8, 4], F32)
        self.sconvb = self.sb("sconvb", [128, L, 8], F32)
        self.fconv = self.sb("fconv", [128, L, NFF, 3], F32)
        self.fconvb = self.sb("fconvb", [128, L, NFF], F32)
        self.dtb = self.sb("dtb", [128, L * 8], F32)
        self.Ab = self.sb("Ab", [128, L * 8], F32)
        self.Db = self.sb("Db", [128, L * 8], F32)
        self.cbias = self.sb("cbias", [128, L, 2], F32)
        self.w2k = self.sb("w2k", [128, L, 2, 128], BF16)
        self.w2v = self.sb("w2v", [128, L, 64], BF16)
        self.xT = self.sb("xT", [128, 8, S], BF16)
        self.xTr = [Res(self.xT.t, "xTr%d" % i) for i in range(NT)]
        self.wslot = [self.sb("wslot%d" % i, [128, 4096], BF16) for i in range(3)]
        self.xtile = [self.sb("xtile%d" % i, [128, D], F32) for i in range(2)]
        self.xh = [self.sb("xh%d" % i, [128, 512], F32) for i in range(5)]
        self.xn = [self.sb("xn%d" % i, [128, D], BF16) for i in range(2)]
        self.nst = [self.sb("nst%d" % i, [128, 2], F32) for i in range(2)]
        self.junkb = self.sb("junkb", [128, D], BF16)
        self.zl = self.sb("zl", [128, 128], BF16)
        self.zr = self.sb("zr", [128, 260], BF16)
        p.dve(lambda e: e.memset(self.zl.t[:, :], 0.0), w=[self.zl])
        p.dve(lambda e: e.memset(self.zr.t[:, :], 0.0), w=[self.zr])
        self.dma(self.identb.t[:, :], d["c_identb"], w=[self.identb])
        self.dma(self.ovl.t[:, :], d["c_ovl"], w=[self.ovl])
        self.dma(self.nfw.t[:, :], d["norm_final_w"].partition_broadcast(128), w=[self.nfw])
        self.dma(self.dtb.t[:, :], d["ssd_dt_bias"].rearrange("l h -> (l h)").partition_broadcast(128), w=[self.dtb])
        self.dma(self.Ab.t[:, :], d["ssd_A_log"].rearrange("l h -> (l h)").partition_broadcast(128), w=[self.Ab])
        self.dma(self.Db.t[:, :], d["ssd_D"].rearrange("l h -> (l h)").partition_broadcast(128), w=[self.Db])
        self.actf(self.Ab.t[:, :], self.Ab.t[:, :], AF.Exp, r=[self.Ab], w=[self.Ab])
        self.ts(self.Ab.t[:, :], self.Ab.t[:, :], -1.0, None, ALU.mult, r=[self.Ab], w=[self.Ab])
        for l in range(L):
            for k in range(4):
                self.dma(self.sconv.t[:, l, :, k], d["ssd_conv_w"][l, k].rearrange("(c p) -> p c", p=128), w=[self.sconv], slow=True)
            self.dma(self.sconvb.t[:, l, :], d["ssd_conv_b"][l].rearrange("(c p) -> p c", p=128), w=[self.sconvb], slow=True)
            for k in range(3):
                self.dma(self.fconv.t[:, l, :, k], d["ffn_conv_w"][l, k].rearrange("(c p) -> p c", p=128), w=[self.fconv], slow=True)
            self.dma(self.fconvb.t[:, l, :], d["ffn_conv_b"][l].rearrange("(c p) -> p c", p=128), w=[self.fconvb], slow=True)
        p.pool(lambda e: e.memset(self.w2k.t[:, :, :, :], 0.0), w=[self.w2k])
        m0 = self.ar.off
        self.prologue()
        p.dve(lambda e: e.memset(self.junkb.t[:, 0:1], 0.0), r=[self.scr] + self.pro_res, w=[self.scr, self.junkb] + self.xTr)
        self.ar.off = m0
        xs = lambda tt: d["xs_s"][tt * 128:(tt + 1) * 128, :]
        for s in range(self.nseq):
            xin = lambda tt, s=s: d["x"][s, tt * 128:(tt + 1) * 128, :]
            for l in range(self.nlayer):
                src = xin if l == 0 else xs
                import os as _os
                ph = _os.environ.get("MKPH", "ssd,nsa,ffn").split(",")
                self.norm_T(src)
                if "ssd" in ph:
                    self.ssd_phase(l, src, xs)
                if "nsa" in ph:
                    self.nsa_phase(l, xs, xs)
                self.norm_T(xs)
                if "ffn" in ph:
                    self.ffn_phase(l, xs, xs)
            for tt in range(NT):
                xt = self.xtile[tt % 2]
                self.dma(xt.t[:, :], xs(tt), r=self.xsr[tt], w=[xt], q=XQ)
                st = self.nst[tt % 2]
                p.dve(lambda e, o_=st.t[:, 0:1]: e.memset(o_, 0.0), w=[st])
                self.actf(self.junkb.t[:, :], xt.t[:, :], AF.Square, r=[xt, st], w=[self.junkb, st], accum=st.t[:, 0:1])
                self.rms_scale(st.t[:, 0:1], st.t[:, 1:2], D, r=[st], w=[st])
                self.ts(xt.t[:, :], xt.t[:, :], st.t[:, 1:2], None, ALU.mult, r=[xt, st], w=[xt])
                self.tt(xt.t[:, :], xt.t[:, :], self.nfw.t[:, :], ALU.mult, r=[xt, self.nfw], w=[xt])
                self.p.dma(d["out"][s, tt * 128:(tt + 1) * 128, :], xt.t[:, :], r=[xt], w=[], q=XQ, out_final=True)
        p.finish()
        return nc


_CACHE = {}


def kernel(**inputs):
    ncores = 8
    nseq = 4
    key = (nseq, L)
    if key not in _CACHE:
        _CACHE[key] = K(nseq, L).build()
    nc = _CACHE[key]
    consts = host_consts()
    x = np.ascontiguousarray(inputs["x"], dtype=np.float32)
    in_maps = []
    for c in range(ncores):
        m = {"x": np.ascontiguousarray(x[c * nseq:(c + 1) * nseq])}
        for k in WEIGHT_SHAPES:
            m[k] = np.ascontiguousarray(np.asarray(inputs[k], dtype=np.float32))
        m.update(consts)
        in_maps.append(m)
    res = run_bass_kernel_spmd(nc, in_maps, core_ids=list(range(ncores)))
    return np.concatenate([np.asarray(r["out"]) for r in res.results], axis=0).astype(np.float32)
```

```python
from contextlib import ExitStack
import numpy as np
import ml_dtypes
import concourse.bass as bass
import concourse.mybir as mybir
from concourse.bass_utils import run_bass_kernel_spmd

F32 = mybir.dt.float32
BF16 = mybir.dt.bfloat16
AF = mybir.ActivationFunctionType
ALU = mybir.AluOpType
AX = mybir.AxisListType


class Res:
    __slots__ = ("t", "name", "lw", "rd")

    def __init__(self, t, name):
        self.t = t
        self.name = name
        self.lw = None
        self.rd = {}

    def sub(self, n):
        return [Res(self.t, "%s_%d" % (self.name, i)) for i in range(n)]


class Prog:
    ENGS = ("pe", "act", "dve", "pool", "sp")

    def __init__(self, nc):
        self.nc = nc
        self.stack = ExitStack()
        self.ops = []
        self.by_eng = {e: [] for e in self.ENGS}
        self.out_ops = []

    def sb(self, name, shape, dt):
        t = self.stack.enter_context(self.nc.sbuf_tensor(name, list(shape), dt))
        return Res(t, name)

    def ps(self, name, shape, dt):
        t = self.stack.enter_context(self.nc.psum_tensor(name, list(shape), dt))
        return Res(t, name)

    def _op(self, eng, fn, r, w, semkey):
        deps = set()
        for x in r:
            if x.lw is not None:
                deps.add(x.lw)
        for x in w:
            if x.lw is not None:
                deps.add(x.lw)
            for v in x.rd.values():
                deps.add(v)
        oid = len(self.ops)
        self.ops.append((eng, fn, deps, semkey))
        self.by_eng[eng].append(oid)
        for x in w:
            x.lw = oid
            x.rd = {}
        for x in r:
            if x not in w:
                x.rd[semkey] = oid
        return oid

    def pe(self, fn, r=(), w=()):
        return self._op("pe", fn, r, w, "pe")

    def act(self, fn, r=(), w=()):
        return self._op("act", fn, r, w, "act")

    def dve(self, fn, r=(), w=()):
        return self._op("dve", fn, r, w, "dve")

    def pool(self, fn, r=(), w=()):
        return self._op("pool", fn, r, w, "pool")

    def _dkey(self, r, w):
        for x in list(w) + list(r):
            if x.t is not None:
                return "dma_" + x.name
        return "dma_misc"

    def dma(self, out, in_, r=(), w=(), q="sp", key=None, out_final=False):
        semkey = self._dkey(r, w)
        oid = self._op(q, lambda e: e.dma_start(out=out, in_=in_), r, w, semkey)
        if out_final:
            self.out_ops.append(oid)
        return oid

    def _op_dma_slow(self, out, in_, r, w, q):
        semkey = self._dkey(r, w)
        return self._op(q, lambda e: e.dma_start(out=out, in_=in_, allow_slow_non_contiguous=True), r, w, semkey)

    def finish(self):
        nc = self.nc
        ops = self.ops
        n = len(ops)
        signal = [False] * n
        for (eng, fn, deps, sk) in ops:
            for d in deps:
                if not (sk == "pe" and ops[d][3] == "pe"):
                    signal[d] = True
        for o in self.out_ops:
            signal[o] = True
        for i, op in enumerate(ops):
            if op[3].startswith("dma_"):
                signal[i] = True
        semcnt = {}
        val = [0] * n
        for i, (eng, fn, deps, sk) in enumerate(ops):
            if signal[i]:
                inc = 16 if sk.startswith("dma_") else 1
                semcnt[sk] = semcnt.get(sk, 0) + inc
                val[i] = semcnt[sk]
        sems = {}
        for sk in semcnt:
            sems[sk] = self.stack.enter_context(nc.semaphore(sk))
        self.semcnt = semcnt
        waited = {e: {} for e in self.ENGS}
        by_eng = self.by_eng
        out_ops = self.out_ops

        def emit(eng, e):
            wd = waited[eng]
            for oid in by_eng[eng]:
                _, fn, deps, sk = ops[oid]
                need = {}
                for d in deps:
                    dsk = ops[d][3]
                    if dsk == "pe" and eng == "pe":
                        continue
                    v = val[d]
                    if v > need.get(dsk, 0):
                        need[dsk] = v
                for dsk, v in need.items():
                    if wd.get(dsk, 0) < v:
                        e.wait_ge(sems[dsk], v)
                        wd[dsk] = v
                ins = fn(e)
                if signal[oid]:
                    ins.then_inc(sems[sk], 16 if sk.startswith("dma_") else 1)
            if eng == "sp":
                need = {}
                for o in out_ops:
                    dsk = ops[o][3]
                    need[dsk] = max(need.get(dsk, 0), val[o])
                for dsk, v in need.items():
                    e.wait_ge(sems[dsk], v)

        with nc.Block() as block:
            @block.tensor
            def _(e):
                emit("pe", e)

            @block.scalar
            def _(e):
                emit("act", e)

            @block.vector
            def _(e):
                emit("dve", e)

            @block.gpsimd
            def _(e):
                emit("pool", e)

            @block.sync
            def _(e):
                emit("sp", e)
        self.stack.close()


S = 2048
D = 1024
NT = 16
L = 4
DFF = 2816
NFF = 22
INC = 2848
NEGB = -30000.0
QT0, KC0, VC0, KS0, KW0, XBC0, DTR0, Z0, VS0, VW0, GL0 = 0, 512, 640, 768, 896, 1024, 2048, 2056, 2568, 2696, 2824
WIN_SEGS = ([((g * 4 + r) * 64, 64) for r in range(4) for g in range(2)] +
            [(512, 128), (640, 128), (768, 128), (1024, 128), (1816, 1024), (2840, 8),
             (1304, 512), (896, 128), (1152, 128), (1280, 24)])


def host_consts():
    bf = ml_dtypes.bfloat16
    c = {}
    c["c_identb"] = np.eye(128, dtype=np.float32).astype(bf)
    c["c_identf"] = np.eye(128, dtype=np.float32)
    p = np.arange(128)[:, None]
    cc = np.arange(512)[None, :]
    causal = np.stack([np.where(cc - p >= 128 * jj, 0.0, NEGB) for jj in range(4)], axis=1)
    c["c_causal"] = causal.astype(np.float32).astype(bf)
    band = np.stack([np.where(cc - p < 128 * (4 - m), 0.0, NEGB) for m in range(1, 5)], axis=1)
    c["c_band"] = band.astype(np.float32).astype(bf)
    t = np.arange(S)[None, :]
    ci = np.arange(128)[:, None]
    c["c_cmpb"] = np.where((16 * ci + 31 <= t) & (ci < 127), 0.0, NEGB).astype(np.float32).astype(bf)
    e = np.zeros((32, 16, 128), np.float32)
    for tl in range(16):
        for pp in range(128):
            e[2 * tl + pp // 64, tl, pp] = 1.0
    c["c_eexp"] = e.astype(bf)
    c0 = np.arange(128)[:, None] * 16
    j0 = np.arange(32)[None, :] * 64
    ov = ((c0 < j0 + 64) & (c0 + 32 > j0)).astype(np.float32)
    ov[127] = 0.0
    c["c_ovl"] = ov
    tt = 1024 + np.arange(1024)[:, None]
    cur = tt // 64
    jb = np.arange(32)[None, :]
    valid = jb <= cur
    forced = ((jb == 0) | (jb == cur) | (jb == cur - 1)).astype(np.float32)
    fs = np.where(valid & (forced < 0.5), 0.0, -1e30).astype(np.float32)
    c["c_fsel"] = np.ascontiguousarray(fs.reshape(8, 128, 32).transpose(1, 0, 2))
    fo = (forced * (-NEGB)).astype(np.float32)
    c["c_forc"] = np.ascontiguousarray(fo.reshape(8, 128, 32).transpose(1, 0, 2))
    tk = np.arange(256)[:, None]
    ll = np.arange(256)[None, :]
    tri = (tk <= ll).astype(np.float32)
    c["c_triu"] = np.ascontiguousarray(tri.reshape(2, 128, 256).transpose(1, 0, 2))
    c["c_tri01"] = (np.arange(128)[None, :] >= np.arange(128)[:, None]).astype(np.float32)
    c["c_ones"] = np.ones((128, 128), np.float32)
    return c


CONST_SHAPES = {
    "c_identb": ([128, 128], BF16), "c_identf": ([128, 128], F32), "c_causal": ([128, 4, 512], BF16),
    "c_band": ([128, 4, 512], BF16), "c_cmpb": ([128, 2048], BF16), "c_eexp": ([32, 16, 128], BF16),
    "c_ovl": ([128, 32], F32), "c_fsel": ([128, 8, 32], F32), "c_forc": ([128, 8, 32], F32), "c_triu": ([128, 2, 256], F32),
    "c_tri01": ([128, 128], F32), "c_ones": ([128, 128], F32),
}

WEIGHT_SHAPES = {
    "norm_mix_w": [L, D], "w_in": [L, D, INC], "cmp_pos_k": [L, 32, 64], "cmp_w1_k": [L, 2048, 128],
    "cmp_b1_k": [L, 128], "cmp_w2_k": [L, 128, 64], "cmp_pos_v": [L, 32, 64], "cmp_w1_v": [L, 2048, 128],
    "cmp_b1_v": [L, 128], "cmp_w2_v": [L, 128, 64], "nsa_norm_w": [L, 512], "ssd_conv_w": [L, 4, 1024],
    "ssd_conv_b": [L, 1024], "ssd_dt_bias": [L, 8], "ssd_A_log": [L, 8], "ssd_D": [L, 8],
    "ssd_norm_w": [L, 512], "w_out": [L, D, D], "norm_ffn_w": [L, D], "w_gate": [L, D, DFF],
    "w_up": [L, D, DFF], "ffn_conv_w": [L, 3, DFF], "ffn_conv_b": [L, DFF], "w_down": [L, DFF, D],
    "norm_final_w": [D],
}


import os as _os0
XQ = "sp"
XQS = _os0.environ.get("MKQS", "act")


class Arena:
    def __init__(self, limit=229344 - 64):
        self.off = 16640 + 64
        self.limit = limit

    def alloc(self, nbytes):
        o = (self.off + 63) // 64 * 64
        self.off = o + nbytes
        self.peak = max(getattr(self, "peak", 0), self.off)
        assert self.off <= self.limit, ("SBUF overflow", self.off)
        return o


def _nbytes(shape, dt):
    n = 1
    for s in shape[1:]:
        n *= s
    return n * (4 if dt == F32 else 2)


class K:
    def __init__(self, nseq=4, nlayer=4, dbg=False):
        self.nseq, self.nlayer, self.dbg = nseq, nlayer, dbg
        nc = self.nc = bass.Bass("TRN2", target_bir_lowering=False)
        self.p = Prog(nc)
        self.ar = Arena()
        self.tog = 0
        d = {}
        d["x"] = nc.dram_tensor("x", [nseq, S, D], F32, kind="ExternalInput").ap()
        for k, shp in WEIGHT_SHAPES.items():
            d[k] = nc.dram_tensor(k, shp, F32, kind="ExternalInput").ap()
        for k, (shp, dt) in CONST_SHAPES.items():
            d[k] = nc.dram_tensor(k, shp, dt, kind="ExternalInput").ap()
        d["out"] = nc.dram_tensor("out", [nseq, S, D], F32, kind="ExternalOutput").ap()
        d["win_s"] = nc.dram_tensor("win_s", [L, 128, 8, INC], BF16, kind="Internal").ap()
        d["wout_s"] = nc.dram_tensor("wout_s", [L, 128, 8, D], BF16, kind="Internal").ap()
        d["wg_s"] = nc.dram_tensor("wg_s", [L, 128, 8, DFF], BF16, kind="Internal").ap()
        d["wu_s"] = nc.dram_tensor("wu_s", [L, 128, 8, DFF], BF16, kind="Internal").ap()
        d["wd_s"] = nc.dram_tensor("wd_s", [L, 128, NFF, D], BF16, kind="Internal").ap()
        d["w1_s"] = nc.dram_tensor("w1_s", [L, 2, 128, 32, 128], BF16, kind="Internal").ap()
        d["xs_s"] = nc.dram_tensor("xs_s", [S, D], F32, kind="Internal").ap()
        self.d = d
        self.ndma = 0

    def sb(self, name, shape, dt):
        off = self.ar.alloc(_nbytes(shape, dt))
        t = self.nc.alloc_sbuf_tensor_at(name, list(shape), dt, offset=off)
        return Res(t, name)

    def mm(self, out, lhsT, rhs, start, stop, r, w):
        self.p.pe(lambda e: e.matmul(out, lhsT, rhs, start=start, stop=stop), r=r, w=w)

    def tr(self, out, in_, ident, r, w):
        self.p.pe(lambda e: e.transpose(out, in_, ident), r=r, w=w)

    def actf(self, out, in_, func, r, w, bias=None, scale=None, accum=None):
        kw = {}
        if bias is not None:
            kw["bias"] = bias
        if scale is not None:
            kw["scale"] = scale
        if accum is not None:
            kw["accum_out"] = accum
        self.p.act(lambda e: e.activation(out, in_, func, **kw), r=r, w=w)

    def cp(self, out, in_, r, w, eng=None):
        if eng is None:
            self.tog ^= 1
            eng = "act" if self.tog else "dve"
        if eng == "act":
            self.p.act(lambda e: e.activation(out, in_, AF.Copy), r=r, w=w)
        elif eng == "dve":
            self.p.dve(lambda e: e.tensor_copy(out, in_), r=r, w=w)
        else:
            self.p.pool(lambda e: e.tensor_copy(out, in_), r=r, w=w)

    def ts(self, out, in0, s1, s2, op0, op1=None, r=(), w=(), eng="dve"):
        if op1 is None:
            f = lambda e: e.tensor_scalar(out, in0, s1, s2, op0)
        else:
            f = lambda e: e.tensor_scalar(out, in0, s1, s2, op0, op1)
        getattr(self.p, eng)(f, r=r, w=w)

    def tt(self, out, in0, in1, op, r=(), w=(), eng="dve"):
        getattr(self.p, eng)(lambda e: e.tensor_tensor(out, in0, in1, op), r=r, w=w)

    def stt(self, out, in0, sc, in1, op0, op1, r=(), w=(), eng="dve"):
        getattr(self.p, eng)(lambda e: e.scalar_tensor_tensor(out, in0, sc, in1, op0, op1), r=r, w=w)

    def dma(self, out, in_, r=(), w=(), out_final=False, slow=False, q="sp"):
        if slow:
            self.p._op_dma_slow(out, in_, r, w, q)
        else:
            self.p.dma(out, in_, r=r, w=w, q=q, out_final=out_final)

    def cv(self, out, in_, scale, r, w):
        self.cvi = getattr(self, "cvi", 0) + 1
        k = self.cvi % 3 if scale is None else self.cvi % 2
        if k == 0:
            if scale is None:
                self.p.act(lambda e: e.activation(out, in_, AF.Copy), r=r, w=w)
            else:
                self.p.act(lambda e: e.activation(out, in_, AF.Copy, scale=scale), r=r, w=w)
        else:
            eng = self.p.dve if k == 1 else self.p.pool
            if scale is None:
                eng(lambda e: e.tensor_copy(out, in_), r=r, w=w)
            else:
                eng(lambda e: e.tensor_scalar(out, in_, scale, None, ALU.mult), r=r, w=w)

    def prologue(self):
        p, d = self.p, self.d
        stg = [self.sb("stg%d" % i, [128, 8192], F32) for i in range(2)]
        stb = [self.sb("stb%d" % i, [128, 8192], BF16) for i in range(2)]
        nw = self.sb("nw", [128, L, 3, 8], F32)
        self.blk = 0

        blocks = []

        def prep(src, KC, N, segs, dst, scale, scale_res):
            CB = min(N, (8192 // KC) // 64 * 64)
            srcv = src.rearrange("(kc p) n -> p kc n", p=128)
            tab = []
            o = 0
            for (sc, ln) in segs:
                tab.append((o, sc, ln))
                o += ln
            assert o == N
            for c0 in range(0, N, CB):
                cb = min(CB, N - c0)
                loads = []
                for (ds, ss, ln) in tab:
                    a, b = max(ds, c0), min(ds + ln, c0 + cb)
                    if a < b:
                        loads.append((a - c0, b - c0, srcv[:, :, ss + (a - ds):ss + (b - ds)]))
                blocks.append((KC, cb, loads, scale, scale_res, dst[:, :, c0:c0 + cb]))

        def emit_load(k):
            KC, cb, loads, scale, scale_res, dst = blocks[k]
            sl = k % 2
            fv = stg[sl].t[:, 0:KC * cb].rearrange("p (kc n) -> p kc n", kc=KC)
            for (a, b, src) in loads:
                self.dma(fv[:, :, a:b], src, w=[stg[sl]])

        def emit_conv(k):
            KC, cb, loads, scale, scale_res, dst = blocks[k]
            sl = k % 2
            fv = stg[sl].t[:, 0:KC * cb].rearrange("p (kc n) -> p kc n", kc=KC)
            bv = stb[sl].t[:, 0:KC * cb].rearrange("p (kc n) -> p kc n", kc=KC)
            for kc in range(KC):
                sc = None if scale is None else scale[:, kc:kc + 1]
                self.cv(bv[:, kc, :], fv[:, kc, :], sc, r=[stg[sl]] + ([scale_res] if scale is not None else []), w=[stb[sl]])
            self.dma(dst, bv, r=[stb[sl]])

        for l in range(self.nlayer):
            self.dma(nw.t[:, l, 0, :], d["norm_mix_w"][l].rearrange("(kc p) -> p kc", p=128), w=[nw], slow=True)
            self.dma(nw.t[:, l, 1, 0:4], d["nsa_norm_w"][l].rearrange("(kc p) -> p kc", p=128), w=[nw], slow=True)
            self.dma(nw.t[:, l, 1, 4:8], d["ssd_norm_w"][l].rearrange("(kc p) -> p kc", p=128), w=[nw], slow=True)
            self.dma(nw.t[:, l, 2, :], d["norm_ffn_w"][l].rearrange("(kc p) -> p kc", p=128), w=[nw], slow=True)
        for l in range(self.nlayer):
            prep(d["w_in"][l], 8, INC, WIN_SEGS, d["win_s"][l], nw.t[:, l, 0, :], nw)
            prep(d["w_out"][l], 8, D, [(0, D)], d["wout_s"][l], nw.t[:, l, 1, :], nw)
            prep(d["w_gate"][l], 8, DFF, [(0, DFF)], d["wg_s"][l], nw.t[:, l, 2, :], nw)
            prep(d["w_up"][l], 8, DFF, [(0, DFF)], d["wu_s"][l], nw.t[:, l, 2, :], nw)
            prep(d["w_down"][l], NFF, D, [(0, D)], d["wd_s"][l], None, None)
        emit_load(0)
        for k in range(len(blocks)):
            if k + 1 < len(blocks):
                emit_load(k + 1)
            emit_conv(k)
        self.blk = len(blocks)
        for l in range(self.nlayer):
            for kv, (w1n, posn, b1n, w2n) in enumerate((("cmp_w1_k", "cmp_pos_k", "cmp_b1_k", "cmp_w2_k"),
                                                        ("cmp_w1_v", "cmp_pos_v", "cmp_b1_v", "cmp_w2_v"))):
                sl = self.blk % 2
                self.blk += 1
                fv = stg[sl].t[0:64, 0:4096].rearrange("p (l n) -> p l n", l=32)
                bv = stb[sl].t[0:64, 0:4096].rearrange("p (l n) -> p l n", l=32)
                self.dma(fv, d[w1n][l].rearrange("(l dd) n -> dd l n", dd=64), w=[stg[sl]])
                post = stg[sl].t[0:64, 4096:4128]
                self.dma(post, d[posn][l].rearrange("l dd -> dd l"), w=[stg[sl]], slow=True)
                b1t = stg[sl].t[:, 4200:4201]
                self.dma(b1t, d[b1n][l].rearrange("(n o) -> n o", o=1), w=[stg[sl]])
                w2t = stg[sl].t[:, 4300:4364]
                self.dma(w2t, d[w2n][l], w=[stg[sl]])
                self.p.dve(lambda e, o_=bv, i_=fv: e.tensor_copy(o_, i_), r=[stg[sl]], w=[stb[sl]])
                for hf in range(2):
                    self.dma(d["w1_s"][l, kv, hf * 64:(hf + 1) * 64], bv, r=[stb[sl]])
                ps = self.psf[0]
                for li in range(32):
                    self.mm(ps.t[:, 0:1], fv[:, li, :], post[:, li:li + 1], li == 0, li == 31, r=[stg[sl]], w=[ps])
                self.tt(self.cbias.t[:, l, kv:kv + 1], ps.t[:, 0:1], b1t, ALU.add, r=[ps, stg[sl]], w=[self.cbias])
                if kv == 0:
                    for g in range(2):
                        self.p.dve(lambda e, o_=self.w2k.t[:, l, g, g * 64:(g + 1) * 64], i_=w2t: e.tensor_copy(o_, i_),
                                   r=[stg[sl]], w=[self.w2k])
                else:
                    self.p.dve(lambda e, o_=self.w2v.t[:, l, :], i_=w2t: e.tensor_copy(o_, i_), r=[stg[sl]], w=[self.w2v])
        self.pro_res = stg + stb + [nw]

    def next_ps(self):
        self.psi = (getattr(self, "psi", -1) + 1) % len(self.psf)
        return self.psf[self.psi]

    def next_psb(self):
        self.psbi = (getattr(self, "psbi", -1) + 1) % len(self.psb)
        return self.psb[self.psbi]

    def load_w(self, src, n):
        self.wsi = (getattr(self, "wsi", -1) + 1) % len(self.wslot)
        ws = self.wslot[self.wsi]
        kc = src.shape[1]
        ap = ws.t[:, 0:kc * n].rearrange("p (k n) -> p k n", k=kc)
        self.dma(ap, src, r=[self.scr], w=[ws])
        return ws, ap

    def to_T(self, src_ap, n, dst_ap, r, w):
        pb = self.next_psb()
        for k in range(n):
            self.tr(pb.t[:, k * 128:(k + 1) * 128], src_ap[:, k * 128:(k + 1) * 128], self.identb.t[:, :], r=r + [self.identb], w=[pb])
        self.cp(dst_ap, pb.t[:, 0:n * 128].rearrange("p (k t) -> p k t", k=n), r=[pb], w=w)

    def rms_scale(self, ssq_ap, out_ap, n, r, w):
        self.ts(out_ap, ssq_ap, 1.0 / n, 1e-6, ALU.mult, ALU.add, r=r, w=w)
        self.actf(out_ap, out_ap, AF.Ln, r=w, w=w)
        self.actf(out_ap, out_ap, AF.Exp, r=w, w=w, scale=-0.5)

    def norm_T(self, xsrc):
        for tt in range(NT):
            xt = self.xtile[tt % 2]
            self.dma(xt.t[:, :], xsrc(tt), r=self.xsr[tt], w=[xt], q=XQ)
            st = self.nst[tt % 2]
            self.p.dve(lambda e, o_=st.t[:, 0:1]: e.memset(o_, 0.0), w=[st])
            self.actf(self.junkb.t[:, :], xt.t[:, :], AF.Square, r=[xt, st], w=[self.junkb, st], accum=st.t[:, 0:1])
            self.rms_scale(st.t[:, 0:1], st.t[:, 1:2], D, r=[st], w=[st])
            xn = self.xn[tt % 2]
            self.ts(xn.t[:, :], xt.t[:, :], st.t[:, 1:2], None, ALU.mult, r=[xt, st], w=[xn])
            self.to_T(xn.t, 8, self.xT.t[:, :, tt * 128:(tt + 1) * 128], r=[xn], w=[self.xTr[tt]])

    def resid_load(self, tt, nh, xsrc):
        self.xhi = (getattr(self, "xhi", -1) + 1) % len(self.xh)
        xh = self.xh[self.xhi]
        self.dma(xh.t[:, :], xsrc(tt)[:, nh * 512:(nh + 1) * 512], r=[self.xsr[tt][nh]], w=[xh], q=XQ)
        return xh

    def resid_fin(self, tt, nh, ps, xh, xdst, lag=2):
        self.tt(xh.t[:, :], xh.t[:, :], ps.t[:, :], ALU.add, r=[ps, xh], w=[xh])
        if not hasattr(self, "pst"):
            self.pst = []
        self.pst.append((xdst(tt)[:, nh * 512:(nh + 1) * 512], xh, self.xsr[tt][nh]))
        self.flush_stores(lag)

    def flush_stores(self, keep=0):
        pst = getattr(self, "pst", [])
        while len(pst) > keep:
            dst, xh, xr = pst.pop(0)
            self.dma(dst, xh.t[:, :], r=[xh], w=[xr], q="sp")

    def resid_add(self, tt, nh, ps, xsrc, xdst):
        xh = self.resid_load(tt, nh, xsrc)
        self.resid_fin(tt, nh, ps, xh, xdst)

    def ssd_phase(self, l, xsrc, xdst):
        p, d = self.p, self.d
        m0 = self.ar.off
        xs_tok = self.sb("xs_tok", [128, NT, 512], BF16)
        B_tok = self.sb("B_tok", [128, NT, 256], BF16)
        BT = self.sb("BT", [128, 2, S], BF16)
        CT = self.sb("CT", [128, 2, S], BF16)
        Wzdt = self.sb("Wzdt", [128, 8, 520], BF16)
        Wos = self.sb("Wos", [128, 4, D], BF16)
        zs = [[self.sb("zs%d_%d" % (i, u), [128, 512], BF16) for u in range(2)] for i in range(2)]
        ysb = [[self.sb("ysb%d_%d" % (i, u), [128, 512], BF16) for u in range(2)] for i in range(2)]
        ytmp = [self.sb("ytmp%d" % u, [128, 512], F32) for u in range(2)]
        zexp = [self.sb("zexp%d" % u, [128, 512], F32) for u in range(2)]
        dtt = [[self.sb("dtt%d_%d" % (i, u), [128, 40], F32) for u in range(2)] for i in range(2)]
        xdt = [[self.sb("xdt%d_%d" % (i, u), [128, 8, 64], BF16) for u in range(2)] for i in range(2)]
        xdd = [[self.sb("xdd%d_%d" % (i, u), [128, 8, 64], BF16) for u in range(2)] for i in range(2)]
        EA = [self.sb("EA%d" % i, [128, 256], F32) for i in range(3)]
        sgm = [[self.sb("sgm%d_%d" % (i, v), [128, 256], F32) for v in range(2)] for i in range(3)]
        MT = [[self.sb("MT%d_%d" % (i, v), [128, 256], BF16) for v in range(2)] for i in range(3)]
        CeT = [self.sb("CeT%d" % i, [128, 256], BF16) for i in range(3)]
        Sst = self.sb("Sst", [128, 2, 256], F32)
        Sbf = self.sb("Sbf", [128, 2, 256], BF16)
        ynb = [self.sb("ynb%d" % u, [128, 512], BF16) for u in range(2)]
        yT = [self.sb("yT%d" % u, [128, 4, 128], BF16) for u in range(2)]
        yst = [self.sb("yst%d" % u, [128, 4], F32) for u in range(2)]
        triu = self.sb("triu", [128, 2, 256], F32)
        tri01 = self.sb("tri01", [128, 128], F32)
        ones = self.sb("ones", [128, 128], F32)
        m1 = self.ar.off
        ctmp = self.sb("ctmp", [128, 1024 + 3], F32)
        cacc = self.sb("cacc", [128, 1024], F32)
        fmT = self.sb("fmT", [128, S], BF16)
        self.ar.off = m1
        Arep = [self.sb("Arep%d" % u, [128, 8, 128], F32) for u in range(2)]
        cbm = [self.sb("cbm%d" % g, [128, 2, 256], F32) for g in range(2)]
        self.ar.off = max(self.ar.off, m1 + 4112 + 4096 + 4096 + 192)
        p.dve(lambda e: e.memset(ctmp.t[:, 0:3], 0.0), w=[ctmp])
        for ch in range(8):
            if ch % 4 == 0:
                ws, wap = self.load_w(d["win_s"][l][:, :, XBC0 + ch * 128:XBC0 + ch * 128 + 512], 512)
            if ch == 1:
                self.dma(triu.t[:, :, :], d["c_triu"], w=[triu])
                self.dma(tri01.t[:, :], d["c_tri01"], w=[tri01])
                self.dma(ones.t[:, :], d["c_ones"], w=[ones])
                self.dma(Wzdt.t[:, :, :], d["win_s"][l][:, :, DTR0:DTR0 + 520], r=[self.scr], w=[Wzdt])
                self.dma(Wos.t[:, :, :], d["wout_s"][l][:, 4:8, :], r=[self.scr], w=[Wos])
            if ch < 4:
                dst, dres = fmT.t[:, :], fmT
            elif ch < 6:
                dst, dres = BT.t[:, ch - 4, :], BT
            else:
                dst, dres = CT.t[:, ch - 6, :], CT
            cw = self.sconv.t[:, l, ch, :]
            for hf in range(2):
                if hf == 0:
                    p.dve(lambda e: e.memset(ctmp.t[:, 0:3], 0.0), w=[ctmp])
                else:
                    self.cp(ctmp.t[:, 0:3], ctmp.t[:, 1024:1027], r=[ctmp], w=[ctmp], eng="dve")
                for I2 in range(2):
                    I = hf * 2 + I2
                    ps = self.next_ps()
                    for kc in range(8):
                        self.mm(ps.t[:, :], wap[:, kc, (ch % 4) * 128:(ch % 4 + 1) * 128], self.xT.t[:, kc, I * 512:(I + 1) * 512],
                                kc == 0, kc == 7, r=[ws] + self.xTr[4 * I:4 * I + 4], w=[ps])
                    self.cp(ctmp.t[:, 3 + I2 * 512:3 + (I2 + 1) * 512], ps.t[:, :], r=[ps], w=[ctmp])
                self.actf(cacc.t[:, :], ctmp.t[:, 3:3 + 1024], AF.Identity, r=[ctmp, self.sconv, self.sconvb], w=[cacc],
                          scale=cw[:, 3:4], bias=self.sconvb.t[:, l, ch:ch + 1])
                for k in range(3):
                    self.stt(cacc.t[:, :], ctmp.t[:, k:k + 1024], cw[:, k:k + 1], cacc.t[:, :], ALU.mult, ALU.add,
                             r=[ctmp, cacc, self.sconv], w=[cacc], eng="dve")
                self.actf(dst[:, hf * 1024:(hf + 1) * 1024], cacc.t[:, :], AF.Silu, r=[cacc], w=[dres])
            if ch < 6:
                for h8 in range(2):
                    pb = self.next_psb()
                    for k in range(8):
                        tk = h8 * 8 + k
                        self.tr(pb.t[:, k * 128:(k + 1) * 128], dst[:, tk * 128:(tk + 1) * 128], self.identb.t[:, :],
                                r=[dres, self.identb], w=[pb])
                    if ch < 4:
                        o_ = xs_tok.t[:, h8 * 8:h8 * 8 + 8, ch * 128:(ch + 1) * 128]
                        ores = xs_tok
                    else:
                        o_ = B_tok.t[:, h8 * 8:h8 * 8 + 8, (ch - 4) * 128:(ch - 3) * 128]
                        ores = B_tok
                    self.cp(o_, pb.t[:, :].rearrange("p (k t) -> p k t", k=8), r=[pb], w=[ores])
        p.dve(lambda e: e.memset(self.junkb.t[:, 0:1], 0.0), r=[ctmp, cacc, fmT], w=[self.junkb] + Arep + cbm)
        p.dve(lambda e: e.memset(Sst.t[:, :, :], 0.0), w=[Sst])
        p.dve(lambda e: e.memset(Sbf.t[:, :, :], 0.0), w=[Sbf])
        dtb, Ab, Db = self.dtb.t[:, l * 8:(l + 1) * 8], self.Ab.t[:, l * 8:(l + 1) * 8], self.Db.t[:, l * 8:(l + 1) * 8]
        prm = [self.dtb, self.Ab, self.Db]
        small = self.psf[5]
        psy = [self.psf[3], self.psf[4]]

        def P1(c):
            for u in range(2):
                tt = 2 * c + u
                z_ = zs[c % 2][u]
                xcols = self.xT.t[:, :, tt * 128:(tt + 1) * 128]
                psz = self.next_ps5()
                for kc in range(8):
                    self.mm(psz.t[:, :], xcols[:, kc, :], Wzdt.t[:, kc, 8:520], kc == 0, kc == 7, r=[Wzdt, self.xTr[tt]], w=[psz])
                for kc in range(8):
                    self.mm(small.t[:, u * 8:u * 8 + 8], xcols[:, kc, :], Wzdt.t[:, kc, 0:8], kc == 0, kc == 7,
                            r=[Wzdt, self.xTr[tt]], w=[small])
                ze = zexp[u]
                self.actf(ze.t[:, :], psz.t[:, :], AF.Exp, r=[psz], w=[ze], scale=-1.0)
                self.ts(ze.t[:, :], ze.t[:, :], 1.0, None, ALU.add, r=[ze], w=[ze])
                self.p.dve(lambda e, o_=ze.t[:, :]: e.reciprocal(o_, o_), r=[ze], w=[ze])
                self.tt(z_.t[:, :], ze.t[:, :], psz.t[:, :], ALU.mult, r=[ze, psz], w=[z_])
                dq = dtt[c % 2][u]
                self.tt(dq.t[:, 0:8], small.t[:, u * 8:u * 8 + 8], dtb, ALU.add, r=[small] + prm, w=[dq])
                self.actf(dq.t[:, 0:8], dq.t[:, 0:8], AF.Exp, r=[dq], w=[dq])
                self.actf(dq.t[:, 0:8], dq.t[:, 0:8], AF.Ln, r=[dq], w=[dq], bias=1.0)
                self.tt(dq.t[:, 8:16], dq.t[:, 0:8], Ab, ALU.mult, r=[dq] + prm, w=[dq])

        def P1b(c):
            for u in range(2):
                tt = 2 * c + u
                dq = dtt[c % 2][u]
                self.tt(xdt[c % 2][u].t[:, :, :], xs_tok.t[:, tt, :].rearrange("p (h e) -> p h e", h=8),
                        dq.t[:, 0:8].unsqueeze(2).to_broadcast([128, 8, 64]), ALU.mult, r=[xs_tok, dq], w=[xdt[c % 2][u]])
                self.p.dve(lambda e, o_=Arep[u].t[:, :, :], i_=dq.t[:, 8:16].unsqueeze(2).to_broadcast([128, 8, 128]):
                           e.tensor_copy(o_, i_), r=[dq], w=[Arep[u]])
            for u in range(2):
                for kt in range(u + 1):
                    self.mm(small.t[:, 16 + u * 8:24 + u * 8], triu.t[:, kt, u * 128:(u + 1) * 128], dtt[c % 2][kt].t[:, 8:16],
                            kt == 0, kt == u, r=[triu, dtt[c % 2][kt]], w=[small])
            for kt in range(2):
                self.mm(small.t[:, 32:40], ones.t[:, :], dtt[c % 2][kt].t[:, 8:16], kt == 0, kt == 1, r=[ones, dtt[c % 2][kt]], w=[small])

        def P1c(c):
            for g in range(2):
                pcb = self.next_ps5()
                for v in range(2):
                    self.mm(pcb.t[:, v * 256:(v + 1) * 256], BT.t[:, g, (2 * c + v) * 128:(2 * c + v + 1) * 128],
                            CT.t[:, g, c * 256:(c + 1) * 256], True, True, r=[BT, CT], w=[pcb])
                self.cp(cbm[g].t[:, :, :], pcb.t[:, :].rearrange("p (v n) -> p v n", v=2), r=[pcb], w=[cbm[g]], eng="act")
                for v in range(2):
                    self.tt(cbm[g].t[:, v, v * 128:(v + 1) * 128], cbm[g].t[:, v, v * 128:(v + 1) * 128], tri01.t[:, :],
                            ALU.mult, r=[cbm[g], tri01], w=[cbm[g]])

        def P2(c):
            for u in range(2):
                dq = dtt[c % 2][u]
                self.cp(dq.t[:, 16:24], small.t[:, 16 + u * 8:24 + u * 8], r=[small], w=[dq], eng="dve")
                self.tt(dq.t[:, 24:32], small.t[:, 32:40], dq.t[:, 16:24], ALU.subtract, r=[small, dq], w=[dq])
                self.cp(dq.t[:, 32:40], small.t[:, 32:40], r=[small], w=[dq], eng="dve")
                self.actf(dq.t[:, 24:40], dq.t[:, 24:40], AF.Exp, r=[dq], w=[dq])
                self.tt(xdd[c % 2][u].t[:, :, :], xdt[c % 2][u].t[:, :, :], dq.t[:, 24:32].unsqueeze(2).to_broadcast([128, 8, 64]),
                        ALU.mult, r=[xdt[c % 2][u], dq], w=[xdd[c % 2][u]])

        def heads(c, hooks):
            prs = {}

            def S1(h):
                pr = self.next_ps5()
                prs[h] = pr
                for kt in range(2):
                    self.mm(pr.t[:, 0:256], Arep[kt].t[:, h, :], triu.t[:, kt, :], kt == 0, kt == 1, r=[Arep[kt], triu], w=[pr])

            def S2a(h):
                i3 = h % 3
                self.actf(EA[i3].t[:, :], prs[h].t[:, 0:256], AF.Exp, r=[prs[h]], w=[EA[i3]])

            def S2b(h):
                g, i3 = h // 4, h % 3
                pr = prs.pop(h)
                for v in range(2):
                    lo = v * 128
                    self.ts(sgm[i3][v].t[:, lo:256], pr.t[:, lo:256], dtt[c % 2][v].t[:, 16 + h:17 + h], 0.0, ALU.subtract, ALU.min,
                            r=[pr, dtt[c % 2][v], EA[i3]], w=[sgm[i3][v]])
                self.tt(CeT[i3].t[:, :], CT.t[:, g, c * 256:(c + 1) * 256], EA[i3].t[:, :], ALU.mult, r=[CT, EA[i3]], w=[CeT[i3]])

            def S2c(h):
                i3 = h % 3
                for v in range(2):
                    lo = v * 128
                    self.actf(sgm[i3][v].t[:, lo:256], sgm[i3][v].t[:, lo:256], AF.Exp, r=[sgm[i3][v]], w=[sgm[i3][v]])

            def S2d(h):
                g, i3 = h // 4, h % 3
                for v in range(2):
                    lo = v * 128
                    self.tt(MT[i3][v].t[:, lo:256], sgm[i3][v].t[:, lo:256], cbm[g].t[:, v, lo:256], ALU.mult,
                            r=[sgm[i3][v], cbm[g]], w=[MT[i3][v]])

            def S3(h):
                g, i3 = h // 4, h % 3
                for u in range(2):
                    o_ = psy[u].t[:, h * 64:(h + 1) * 64]
                    for v in range(u + 1):
                        self.mm(o_, MT[i3][v].t[:, u * 128:(u + 1) * 128], xdt[c % 2][v].t[:, h, :], v == 0, False,
                                r=[MT[i3][v], xdt[c % 2][v]], w=[psy[u]])
                    self.mm(o_, CeT[i3].t[:, u * 128:(u + 1) * 128], Sbf.t[:, g, (h % 4) * 64:(h % 4 + 1) * 64], False, True,
                            r=[CeT[i3], Sbf], w=[psy[u]])

            for t in range(8 + 3):
                if 1 <= t <= 8:
                    S2a(t - 1)
                    S2b(t - 1)
                for fn in hooks.get(t, []):
                    fn()
                if t < 8:
                    S1(t)
                if 2 <= t <= 9:
                    S2c(t - 2)
                    S2d(t - 2)
                if t >= 3:
                    S3(t - 3)

        def E(c):
            for u in range(2):
                self.cp(ysb[c % 2][u].t[:, :], psy[u].t[:, :], r=[psy[u]], w=[ysb[c % 2][u]], eng="act")
            for g in range(2):
                pss = self.next_ps5()
                for u in range(2):
                    self.mm(pss.t[:, 0:256], B_tok.t[:, 2 * c + u, g * 128:(g + 1) * 128],
                            xdd[c % 2][u].t[:, 4 * g:4 * g + 4, :].rearrange("p h e -> p (h e)"), u == 0, u == 1,
                            r=[B_tok, xdd[c % 2][u]], w=[pss])
                for r4 in range(4):
                    h = 4 * g + r4
                    self.stt(Sst.t[:, g, r4 * 64:(r4 + 1) * 64], Sst.t[:, g, r4 * 64:(r4 + 1) * 64], dtt[c % 2][0].t[:, 32 + h:33 + h],
                             pss.t[:, r4 * 64:(r4 + 1) * 64], ALU.mult, ALU.add, r=[Sst, dtt[c % 2][0], pss], w=[Sst])
                self.cp(Sbf.t[:, g, :], Sst.t[:, g, :], r=[Sst], w=[Sbf], eng="dve")

        def F(c, u):
            tt = 2 * c + u
            y_ = ytmp[u]
            z_ = zs[c % 2][u]
            self.tt(y_.t[:, :].rearrange("p (h e) -> p h e", h=8), xs_tok.t[:, tt, :].rearrange("p (h e) -> p h e", h=8),
                    Db.unsqueeze(2).to_broadcast([128, 8, 64]), ALU.mult, r=[xs_tok] + prm, w=[y_])
            self.tt(y_.t[:, :], y_.t[:, :], ysb[c % 2][u].t[:, :], ALU.add, r=[y_, ysb[c % 2][u]], w=[y_])
            self.tt(y_.t[:, :], y_.t[:, :], z_.t[:, :], ALU.mult, r=[y_, z_], w=[y_])
            self.p.dve(lambda e, o_=yst[u].t[:, 0:2]: e.memset(o_, 0.0), w=[yst[u]])
            for g in range(2):
                self.actf(self.junkb.t[:, 0:256], y_.t[:, g * 256:(g + 1) * 256], AF.Square, r=[y_, yst[u]],
                          w=[self.junkb, yst[u]], accum=yst[u].t[:, g:g + 1])
            self.rms_scale(yst[u].t[:, 0:2], yst[u].t[:, 2:4], 256, r=[yst[u]], w=[yst[u]])
            for g in range(2):
                self.ts(ynb[u].t[:, g * 256:(g + 1) * 256], y_.t[:, g * 256:(g + 1) * 256], yst[u].t[:, 2 + g:3 + g], None,
                        ALU.mult, r=[y_, yst[u]], w=[ynb[u]])

        def Fb(c, u):
            tt = 2 * c + u
            self.to_T(ynb[u].t, 4, yT[u].t[:, :, :], r=[ynb[u]], w=[yT[u]])
            for nh in range(2):
                po = self.next_ps5()
                xh = self.resid_load(tt, nh, xsrc)
                for k4 in range(4):
                    self.mm(po.t[:, :], yT[u].t[:, k4, :], Wos.t[:, k4, nh * 512:(nh + 1) * 512], k4 == 0, k4 == 3,
                            r=[yT[u], Wos], w=[po])
                self.resid_fin(tt, nh, po, xh, xdst)

        P1(0)
        P1b(0)
        P1c(0)
        P2(0)
        for c in range(8):
            hooks = {}
            if c >= 1:
                hooks.setdefault(1, []).append(lambda c=c: F(c - 1, 0))
                hooks.setdefault(2, []).append(lambda c=c: F(c - 1, 1))
                hooks.setdefault(5, []).append(lambda c=c: Fb(c - 1, 0))
                hooks.setdefault(8, []).append(lambda c=c: Fb(c - 1, 1))
            if c < 7:
                hooks.setdefault(6, []).append(lambda c=c: P1(c + 1))
                hooks.setdefault(9, []).append(lambda c=c: P1b(c + 1))
                hooks.setdefault(10, []).append(lambda c=c: (P1c(c + 1), P2(c + 1)))
            heads(c, hooks)
            E(c)
        F(7, 0)
        F(7, 1)
        Fb(7, 0)
        Fb(7, 1)
        self.flush_stores(0)
        self.ar.off = m0

    def next_ps5(self):
        self.ps5i = (getattr(self, "ps5i", -1) + 1) % 3
        return self.psf[self.ps5i]

    def nsa_phase(self, l, xsrc, xdst):
        p, d = self.p, self.d
        m0 = self.ar.off
        qT = self.sb("qT", [128, 4, S], BF16)
        kT = [self.sb("kT%d" % i, [128, S], BF16) for i in range(4)]
        vsa = self.sb("vsa", [128, NT, 2, 65], BF16)
        vwa = self.sb("vwa", [128, NT, 2, 65], BF16)
        gates = self.sb("gates", [128, NT, 24], F32)
        w1 = [self.sb("w1_%d" % i, [128, 32, 128], BF16) for i in range(2)]
        causal = self.sb("causal", [128, 4, 512], BF16)
        band = self.sb("band", [128, 4, 512], BF16)
        cmpb = self.sb("cmpb", [128, S], BF16)
        eexp = self.sb("eexp", [32, NT, 128], BF16)
        fsel = self.sb("fsel", [128, 8, 32], F32)
        forc = self.sb("forc", [128, 8, 32], F32)
        Woa = self.sb("Woa", [128, 4, D], BF16)
        kcmpT = self.sb("kcmpT", [128, 128], BF16)
        rhsc = self.sb("rhsc", [128, 2, 97], F32)
        hx = [self.sb("hx%d" % i, [128, 128], F32) for i in range(3)]
        hidb = [self.sb("hidb%d" % g, [128, 128], BF16) for g in range(2)]
        selbT = self.sb("selbT", [32, 2, 1024], BF16)
        ocur = self.sb("ocur", [128, 4, 512], F32)
        PT = [self.sb("PT%d" % i, [128, 512], BF16) for i in range(4)]
        PcT = [self.sb("PcT%d" % i, [128, 512], F32) for i in range(2)]
        impacc = [self.sb("impacc%d" % g, [128, 4, 32], F32) for g in range(2)]
        imptmp = self.sb("imptmp", [128, 4, 32], F32)
        sm = [self.sb("sm%d" % i, [128, 16], F32) for i in range(2)]
        sc = self.sb("sc", [128, 32], F32)
        wk = self.sb("wk", [128, 32], F32)
        m8 = self.sb("m8", [128, 16], F32)
        selb = self.sb("selb", [128, 32], BF16)
        onb = [self.sb("onb%d" % i, [128, 512], BF16) for i in range(2)]
        ost = [self.sb("ost%d" % i, [128, 2], F32) for i in range(2)]
        p.dve(lambda e: e.memset(vsa.t[:, :, :, 64:65], 1.0), w=[vsa])
        p.dve(lambda e: e.memset(vwa.t[:, :, :, 64:65], 1.0), w=[vwa])
        p.dve(lambda e: e.memset(kcmpT.t[:, :], 0.0), w=[kcmpT])
        p.dve(lambda e: e.memset(rhsc.t[:, :, :], 0.0), w=[rhsc])
        for g in range(2):
            p.dve(lambda e, g=g: e.memset(hidb[g].t[:, :], 0.0), w=[hidb[g]])
            p.dve(lambda e, g=g: e.tensor_copy(rhsc.t[:, g, 0:32], self.ovl.t[:, :]), r=[self.ovl], w=[rhsc])
            p.dve(lambda e, g=g: e.memset(rhsc.t[:, g, 96:97], 1.0), w=[rhsc])
        fm_dst = [(qT, qT.t[:, 0, :]), (qT, qT.t[:, 1, :]), (qT, qT.t[:, 2, :]), (qT, qT.t[:, 3, :]),
                  (kT[0], kT[0].t[:, :]), (kT[1], kT[1].t[:, :]), (kT[2], kT[2].t[:, :]), (kT[3], kT[3].t[:, :])]
        for ch in range(8):
            if ch % 4 == 0:
                ws, wap = self.load_w(d["win_s"][l][:, :, ch * 128:ch * 128 + 512], 512)
            for I in range(4):
                ps = self.next_ps()
                for kc in range(8):
                    self.mm(ps.t[:, :], wap[:, kc, (ch % 4) * 128:(ch % 4 + 1) * 128], self.xT.t[:, kc, I * 512:(I + 1) * 512],
                            kc == 0, kc == 7, r=[ws] + self.xTr[4 * I:4 * I + 4], w=[ps])
                self.cp(fm_dst[ch][1][:, I * 512:(I + 1) * 512], ps.t[:, :], r=[ps], w=[fm_dst[ch][0]])
        ws, wap = self.load_w(d["win_s"][l][:, :, VS0:VS0 + 280], 280)
        for kv in range(2):
            self.dma(w1[kv].t[:, :, :], d["w1_s"][l, kv], r=[self.scr], w=[w1[kv]])
        for (t_, nm) in ((cmpb, "c_cmpb"), (causal, "c_causal"), (band, "c_band"), (eexp, "c_eexp"), (fsel, "c_fsel"), (forc, "c_forc")):
            self.dma(t_.t[:], d[nm], w=[t_])
        self.dma(Woa.t[:, :, :], d["wout_s"][l][:, 0:4, :], r=[self.scr], w=[Woa])
        for tt in range(NT):
            ps = self.next_ps()
            for kc in range(8):
                self.mm(ps.t[:, 0:280], self.xT.t[:, kc, tt * 128:(tt + 1) * 128], wap[:, kc, :], kc == 0, kc == 7,
                        r=[ws, self.xTr[tt]], w=[ps])
            self.cp(vsa.t[:, tt, :, 0:64], ps.t[:, 0:128].rearrange("p (g e) -> p g e", g=2), r=[ps], w=[vsa], eng="act")
            self.cp(vwa.t[:, tt, :, 0:64], ps.t[:, 128:256].rearrange("p (g e) -> p g e", g=2), r=[ps], w=[vwa], eng="act")
            self.actf(gates.t[:, tt, :], ps.t[:, 256:280], AF.Sigmoid, r=[ps], w=[gates])
        for kv in range(2):
            for g in range(2):
                ps = self.next_ps()
                for li in range(32):
                    self.mm(ps.t[:, 0:127], w1[kv].t[g * 64:(g + 1) * 64, li, :], kT[kv].t[g * 64:(g + 1) * 64, li:li + 16 * 126 + 1:16],
                            li == 0, li == 31, r=[w1[kv], kT[kv]], w=[ps])
                xg, x2, sg = hx[0], hx[1], hx[2]
                self.actf(xg.t[:, 0:127], ps.t[:, 0:127], AF.Identity, r=[ps, self.cbias], w=[xg], bias=self.cbias.t[:, l, kv:kv + 1])
                self.tt(x2.t[:, 0:127], xg.t[:, 0:127], xg.t[:, 0:127], ALU.mult, r=[xg], w=[x2])
                self.ts(x2.t[:, 0:127], x2.t[:, 0:127], 0.044715, 1.0, ALU.mult, ALU.add, r=[x2], w=[x2])
                self.tt(x2.t[:, 0:127], x2.t[:, 0:127], xg.t[:, 0:127], ALU.mult, r=[x2, xg], w=[x2])
                self.actf(sg.t[:, 0:127], x2.t[:, 0:127], AF.Sigmoid, r=[x2], w=[sg], scale=1.5957691216057308)
                self.tt(hidb[g].t[:, 0:127], xg.t[:, 0:127], sg.t[:, 0:127], ALU.mult, r=[xg, sg], w=[hidb[g]])
                if kv == 1:
                    pv = self.next_ps()
                    self.mm(pv.t[0:127, 0:64], hidb[g].t[:, 0:127], self.w2v.t[:, l, :], True, True, r=[hidb[g], self.w2v], w=[pv])
                    self.cp(rhsc.t[0:127, g, 32:96], pv.t[0:127, 0:64], r=[pv], w=[rhsc], eng="dve")
            if kv == 0:
                pk = self.next_ps()
                for g in range(2):
                    self.mm(pk.t[:, 0:127], self.w2k.t[:, l, g, :], hidb[g].t[:, 0:127], g == 0, g == 1, r=[self.w2k, hidb[g]], w=[pk])
                self.cp(kcmpT.t[:, 0:127], pk.t[:, 0:127], r=[pk], w=[kcmpT], eng="dve")
        ksT, kwT = kT[2], kT[3]
        pcb = [self.psf[3], self.psf[4], self.psf[5]]
        SK = 2
        hh = 0
        for I in range(4):
            tiles = slice(4 * I, 4 * I + 4)

            def cmp_score(h):
                g, r4 = h // 4, h % 4
                gp = slice(g * 64, (g + 1) * 64)
                q = qT.t[gp, r4, I * 512:(I + 1) * 512]
                ps = self.next_ps5()
                self.mm(ps.t[:, :], kcmpT.t[gp, :], q, True, False, r=[kcmpT, qT], w=[ps])
                self.mm(ps.t[:, :], self.identb.t[:, :], cmpb.t[:, I * 512:(I + 1) * 512], False, True, r=[self.identb, cmpb], w=[ps])
                self.actf(PcT[h % 2].t[:, :], ps.t[:, :], AF.Exp, r=[ps], w=[PcT[h % 2]], scale=0.125)

            def cmp_pv(h):
                g, r4 = h // 4, h % 4
                pc_ = PcT[h % 2]
                pc = pcb[h % 3]
                for sub in range(4):
                    self.mm(pc.t[:, sub * 97:(sub + 1) * 97], pc_.t[:, sub * 128:(sub + 1) * 128], rhsc.t[:, g, :], True, True,
                            r=[pc_, rhsc], w=[pc])
                pcv = pc.t[:, 0:388].rearrange("p (s n) -> p s n", s=4)
                s_ = sm[h % 2]
                self.ts(s_.t[:, 0:4], pcv[:, :, 96], 1e-30, None, ALU.max, r=[pc], w=[s_])
                self.p.dve(lambda e, o_=s_.t[:, 4:8], i_=s_.t[:, 0:4]: e.reciprocal(o_, i_), r=[s_], w=[s_])
                self.tt(s_.t[:, 8:12], s_.t[:, 4:8], gates.t[:, tiles, h * 3 + 0], ALU.mult, r=[s_, gates], w=[s_])
                rb = s_.t[:, 4:8].unsqueeze(2).to_broadcast([128, 4, 32])
                if r4 == 0:
                    self.tt(impacc[g].t[:, :, :], pcv[:, :, 0:32], rb, ALU.mult, r=[pc, s_], w=[impacc[g]])
                else:
                    self.tt(imptmp.t[:, :, :], pcv[:, :, 0:32], rb, ALU.mult, r=[pc, s_], w=[imptmp])
                    self.tt(impacc[g].t[:, :, :], impacc[g].t[:, :, :], imptmp.t[:, :, :], ALU.add, r=[imptmp, impacc[g]], w=[impacc[g]])
                for sub in range(4):
                    self.ts(ocur.t[:, sub, h * 64:(h + 1) * 64], pcv[:, sub, 32:96], s_.t[:, 8 + sub:9 + sub], None, ALU.mult,
                            r=[pc, s_], w=[ocur])

            for h in range(9):
                if h < 8:
                    cmp_score(h)
                if h >= 1:
                    cmp_pv(h - 1)
            if I >= 2:
                for g in range(2):
                    for sub in range(4):
                        ti = 4 * (I - 2) + sub
                        self.tt(sc.t[:, :], impacc[g].t[:, sub, :], fsel.t[:, ti, :], ALU.add, r=[impacc[g], fsel], w=[sc])
                        self.p.dve(lambda e: e.max(out=m8.t[:, 0:8], in_=sc.t[:, :]), r=[sc], w=[m8])
                        self.p.dve(lambda e: e.match_replace(out=wk.t[:, :], in_to_replace=m8.t[:, 0:8], in_values=sc.t[:, :], imm_value=-3e38),
                                   r=[sc, m8], w=[wk])
                        self.p.dve(lambda e: e.max(out=m8.t[:, 8:16], in_=wk.t[:, :]), r=[wk], w=[m8])
                        self.ts(wk.t[:, :], sc.t[:, :], m8.t[:, 12:13], None, ALU.is_ge, r=[sc, m8], w=[wk])
                        self.ts(wk.t[:, :], wk.t[:, :], -NEGB, None, ALU.mult, r=[wk], w=[wk])
                        self.tt(wk.t[:, :], wk.t[:, :], forc.t[:, ti, :], ALU.add, r=[wk, forc], w=[wk])
                        self.ts(selb.t[:, :], wk.t[:, :], NEGB, 0.0, ALU.add, ALU.min, r=[wk], w=[selb])
                        pb = self.next_psb()
                        self.tr(pb.t[0:32, 0:128], selb.t[:, :], self.identb.t[:, :], r=[selb, self.identb], w=[pb])
                        self.cp(selbT.t[:, g, ti * 128:(ti + 1) * 128], pb.t[0:32, 0:128], r=[pb], w=[selbT], eng="act")
            tl = []
            for br in (1, 0):
                for h in range(8):
                    j0 = 0 if br == 0 else max(0, 4 * I - 4)
                    for j in range(j0, 4 * I + 4):
                        tl.append((br, h, j, j == j0, j == 4 * I + 3, hh))
                    hh += 1
            pts = {}

            def score(ix):
                br, h, j, first, last, hn = tl[ix]
                kk = ksT if br == 0 else kwT
                g, r4 = h // 4, h % 4
                gp = slice(g * 64, (g + 1) * 64)
                mrel = 4 * I - j
                c_lo, c_hi = 0, 512
                extra = []
                if br == 0 and I >= 2:
                    extra.append(("sel", None))
                if mrel <= 0:
                    c_lo = 128 * (-mrel)
                    extra.append(("causal", -mrel))
                elif br == 1:
                    c_hi = 128 * (5 - mrel)
                    extra.append(("band", mrel - 1))
                ps = self.next_ps5()
                q = qT.t[gp, r4, I * 512 + c_lo:I * 512 + c_hi]
                self.mm(ps.t[:, c_lo:c_hi], kk.t[gp, j * 128:(j + 1) * 128], q, True, len(extra) == 0, r=[kk, qT], w=[ps])
                for ei, (kind, idx) in enumerate(extra):
                    lst = ei == len(extra) - 1
                    if kind == "sel":
                        self.mm(ps.t[:, c_lo:c_hi], eexp.t[:, j, :], selbT.t[:, g, (I - 2) * 512 + c_lo:(I - 2) * 512 + c_hi],
                                False, lst, r=[eexp, selbT], w=[ps])
                    elif kind == "causal":
                        self.mm(ps.t[:, c_lo:c_hi], self.identb.t[:, :], causal.t[:, idx, c_lo:c_hi], False, lst,
                                r=[self.identb, causal], w=[ps])
                    else:
                        self.mm(ps.t[:, c_lo:c_hi], self.identb.t[:, :], band.t[:, idx, c_lo:c_hi], False, lst,
                                r=[self.identb, band], w=[ps])
                pt = PT[ix % len(PT)]
                self.actf(pt.t[:, c_lo:c_hi], ps.t[:, c_lo:c_hi], AF.Exp, r=[ps], w=[pt], scale=0.125)
                pts[ix] = (pt, c_lo, c_hi)

            def pv(ix):
                br, h, j, first, last, hn = tl[ix]
                va = vsa if br == 0 else vwa
                g = h // 4
                po = self.psf[3 + hn % 2]
                pt, c_lo, c_hi = pts.pop(ix)
                if first:
                    self.mm(po.t[:, 0:260], self.zl.t[:, :], self.zr.t[:, :], True, False, r=[self.zl, self.zr], w=[po])
                for sub in range(c_lo // 128, c_hi // 128):
                    self.mm(po.t[:, sub * 65:(sub + 1) * 65], pt.t[:, sub * 128:(sub + 1) * 128], va.t[:, j, g, :],
                            False, j == 4 * I + sub, r=[pt, va], w=[po])
                if last:
                    pov = po.t[:, 0:260].rearrange("p (s n) -> p s n", s=4)
                    s_ = sm[hn % 2]
                    self.p.dve(lambda e, o_=s_.t[:, 4:8], i_=pov[:, :, 64]: e.reciprocal(o_, i_), r=[po], w=[s_])
                    self.tt(s_.t[:, 8:12], s_.t[:, 4:8], gates.t[:, tiles, h * 3 + 1 + br], ALU.mult, r=[s_, gates], w=[s_])
                    for sub in range(4):
                        oc = ocur.t[:, sub, h * 64:(h + 1) * 64]
                        self.stt(oc, pov[:, sub, 0:64], s_.t[:, 8 + sub:9 + sub], oc, ALU.mult, ALU.add, r=[po, s_, ocur], w=[ocur])

            for ix in range(len(tl) + SK):
                if ix < len(tl):
                    score(ix)
                if ix >= SK:
                    pv(ix - SK)
            for sub in range(4):
                tt = 4 * I + sub
                st = ost[sub % 2]
                self.p.dve(lambda e, o_=st.t[:, 0:1]: e.memset(o_, 0.0), w=[st])
                self.actf(self.junkb.t[:, 0:512], ocur.t[:, sub, :], AF.Square, r=[ocur, st], w=[self.junkb, st], accum=st.t[:, 0:1])
                self.rms_scale(st.t[:, 0:1], st.t[:, 1:2], 512, r=[st], w=[st])
                ob = onb[sub % 2]
                self.ts(ob.t[:, :], ocur.t[:, sub, :], st.t[:, 1:2], None, ALU.mult, r=[ocur, st], w=[ob])
                self.to_T(ob.t, 4, self.xT.t[:, 0:4, tt * 128:(tt + 1) * 128], r=[ob], w=[self.xTr[tt]])
        for tt in range(NT):
            for nh in range(2):
                po = self.next_ps()
                xh = self.resid_load(tt, nh, xsrc)
                for k4 in range(4):
                    self.mm(po.t[:, :], self.xT.t[:, k4, tt * 128:(tt + 1) * 128], Woa.t[:, k4, nh * 512:(nh + 1) * 512], k4 == 0, k4 == 3,
                            r=[self.xTr[tt], Woa], w=[po])
                self.resid_fin(tt, nh, po, xh, xdst)
        self.flush_stores(0)
        self.ar.off = m0

    def ffn_phase(self, l, xsrc, xdst):
        p, d = self.p, self.d
        m0 = self.ar.off
        act = self.sb("act", [128, NFF, 1024], BF16)
        gbuf = [self.sb("gbuf%d" % i, [128, 1026], F32) for i in range(2)]
        halo = self.sb("halo", [128, NFF, 2], F32)
        cacc = [self.sb("facc%d" % i, [128, 1024], F32) for i in range(2)]
        sact = [self.sb("sact%d" % i, [128, 1024], F32) for i in range(2)]
        NS, DA = 6, 4
        fsl = [self.sb("fsl%d" % i, [128, 4096], BF16) for i in range(NS)]
        blocks = []
        for half in range(2):
            for f0 in range(0, NFF, 4):
                n = min(512, DFF - f0 * 128)
                blocks.append((d["wg_s"][l][:, :, f0 * 128:f0 * 128 + n], 8, n))
                blocks.append((d["wu_s"][l][:, :, f0 * 128:f0 * 128 + n], 8, n))
            for tg in range(2):
                for nh in range(2):
                    for f0 in (0, 8, 16):
                        nf = min(8, NFF - f0)
                        blocks.append((d["wd_s"][l][:, f0:f0 + nf, nh * 512:(nh + 1) * 512], nf, 512))
        state = {"emitted": 0, "k": 0}

        def get():
            k = state["k"]
            state["k"] += 1
            while state["emitted"] < min(len(blocks), k + DA + 1):
                e = state["emitted"]
                src, kc, n = blocks[e]
                ws = fsl[e % NS]
                self.dma(ws.t[:, 0:kc * n].rearrange("p (k n) -> p k n", k=kc), src, r=[self.scr], w=[ws])
                state["emitted"] += 1
            src, kc, n = blocks[k]
            ws = fsl[k % NS]
            return ws, ws.t[:, 0:kc * n].rearrange("p (k n) -> p k n", k=kc)

        for half in range(2):
            t0 = half * 1024
            for f in range(NFF):
                if f % 4 == 0:
                    wsg, wg = get()
                    wsu, wu = get()
                fc = slice((f % 4) * 128, (f % 4 + 1) * 128)
                gb, ca, sa = gbuf[f % 2], cacc[f % 2], sact[f % 2]
                if half == 0:
                    p.dve(lambda e, o_=gb.t[:, 0:2]: e.memset(o_, 0.0), w=[gb])
                else:
                    self.cp(gb.t[:, 0:2], halo.t[:, f, :], r=[halo], w=[gb], eng="dve")
                for I2 in range(2):
                    ps = self.next_ps()
                    for kc in range(8):
                        self.mm(ps.t[:, :], wg[:, kc, fc], self.xT.t[:, kc, t0 + I2 * 512:t0 + (I2 + 1) * 512], kc == 0, kc == 7,
                                r=[wsg] + self.xTr[(t0 // 128) + 4 * I2:(t0 // 128) + 4 * I2 + 4], w=[ps])
                    self.cp(gb.t[:, 2 + I2 * 512:2 + (I2 + 1) * 512], ps.t[:, :], r=[ps], w=[gb])
                if half == 0:
                    self.cp(halo.t[:, f, :], gb.t[:, 1024:1026], r=[gb], w=[halo], eng="dve")
                cw = self.fconv.t[:, l, f, :]
                self.actf(ca.t[:, :], gb.t[:, 2:1026], AF.Identity, r=[gb, self.fconv, self.fconvb], w=[ca],
                          scale=cw[:, 2:3], bias=self.fconvb.t[:, l, f:f + 1])
                self.stt(ca.t[:, :], gb.t[:, 1:1025], cw[:, 1:2], ca.t[:, :], ALU.mult, ALU.add, r=[gb, ca, self.fconv], w=[ca], eng="dve")
                self.stt(ca.t[:, :], gb.t[:, 0:1024], cw[:, 0:1], ca.t[:, :], ALU.mult, ALU.add, r=[gb, ca, self.fconv], w=[ca], eng="dve")
                self.actf(sa.t[:, :], ca.t[:, :], AF.Silu, r=[ca], w=[sa])
                for I2 in range(2):
                    ps = self.next_ps()
                    for kc in range(8):
                        self.mm(ps.t[:, :], wu[:, kc, fc], self.xT.t[:, kc, t0 + I2 * 512:t0 + (I2 + 1) * 512], kc == 0, kc == 7,
                                r=[wsu] + self.xTr[(t0 // 128) + 4 * I2:(t0 // 128) + 4 * I2 + 4], w=[ps])
                    self.tt(act.t[:, f, I2 * 512:(I2 + 1) * 512], sa.t[:, I2 * 512:(I2 + 1) * 512], ps.t[:, :], ALU.mult,
                            r=[sa, ps], w=[act])
            for tg in range(2):
                for nh in range(2):
                    accs = [self.next_ps() for _ in range(4)]
                    self.flush_stores(0)
                    xhs = [self.resid_load(half * 8 + tg * 4 + ti, nh, xsrc) for ti in range(4)]
                    for f0 in (0, 8, 16):
                        nf = min(8, NFF - f0)
                        wsd, wd = get()
                        for ti in range(4):
                            tl = tg * 4 + ti
                            for fi in range(nf):
                                f = f0 + fi
                                self.mm(accs[ti].t[:, :], act.t[:, f, tl * 128:(tl + 1) * 128], wd[:, fi, :], f == 0, f == NFF - 1,
                                        r=[act, wsd], w=[accs[ti]])
                    for ti in range(4):
                        self.resid_fin(half * 8 + tg * 4 + ti, nh, accs[ti], xhs[ti], xdst, lag=4)
        self.flush_stores(0)
        self.ar.off = m0

    def build(self):
        p, d, nc = self.p, self.d, self.nc
        self.psf = [Res(p.stack.enter_context(nc.psum_tensor("psf%d" % i, [128, 512], F32)), "psf%d" % i) for i in range(6)]
        self.psb = [Res(p.stack.enter_context(nc.psum_tensor("psb%d" % i, [128, 1024], BF16)), "psb%d" % i) for i in range(2)]
        self.scr = Res(None, "scr")
        self.xsr = [[Res(None, "xsr%d_%d" % (i, j)) for j in range(2)] for i in range(NT)]
        self.identb = self.sb("identb", [128, 128], BF16)
        self.ovl = self.sb("ovl", [128, 32], F32)
        self.nfw = self.sb("nfw", [128, D], F32)
        self.sconv = self.sb("sconv", [128, L, 8, 4], F32)
        self.sconvb = self.sb("sconvb", [128, L, 8], F32)
        self.fconv = self.sb("fconv", [128, L, NFF, 3], F32)
        self.fconvb = self.sb("fconvb", [128, L, NFF], F32)
        self.dtb = self.sb("dtb", [128, L * 8], F32)
        self.Ab = self.sb("Ab", [128, L * 8], F32)
        self.Db = self.sb("Db", [128, L * 8], F32)
        self.cbias = self.sb("cbias", [128, L, 2], F32)
        self.w2k = self.sb("w2k", [128, L, 2, 128], BF16)
        self.w2v = self.sb("w2v", [128, L, 64], BF16)
        self.xT = self.sb("xT", [128, 8, S], BF16)
        self.xTr = [Res(self.xT.t, "xTr%d" % i) for i in range(NT)]
        self.wslot = [self.sb("wslot%d" % i, [128, 4096], BF16) for i in range(3)]
        self.xtile = [self.sb("xtile%d" % i, [128, D], F32) for i in range(2)]
        self.xh = [self.sb("xh%d" % i, [128, 512], F32) for i in range(5)]
        self.xn = [self.sb("xn%d" % i, [128, D], BF16) for i in range(2)]
        self.nst = [self.sb("nst%d" % i, [128, 2], F32) for i in range(2)]
        self.junkb = self.sb("junkb", [128, D], BF16)
        self.zl = self.sb("zl", [128, 128], BF16)
        self.zr = self.sb("zr", [128, 260], BF16)
        p.dve(lambda e: e.memset(self.zl.t[:, :], 0.0), w=[self.zl])
        p.dve(lambda e: e.memset(self.zr.t[:, :], 0.0), w=[self.zr])
        self.dma(self.identb.t[:, :], d["c_identb"], w=[self.identb])
        self.dma(self.ovl.t[:, :], d["c_ovl"], w=[self.ovl])
        self.dma(self.nfw.t[:, :], d["norm_final_w"].partition_broadcast(128), w=[self.nfw])
        self.dma(self.dtb.t[:, :], d["ssd_dt_bias"].rearrange("l h -> (l h)").partition_broadcast(128), w=[self.dtb])
        self.dma(self.Ab.t[:, :], d["ssd_A_log"].rearrange("l h -> (l h)").partition_broadcast(128), w=[self.Ab])
        self.dma(self.Db.t[:, :], d["ssd_D"].rearrange("l h -> (l h)").partition_broadcast(128), w=[self.Db])
        self.actf(self.Ab.t[:, :], self.Ab.t[:, :], AF.Exp, r=[self.Ab], w=[self.Ab])
        self.ts(self.Ab.t[:, :], self.Ab.t[:, :], -1.0, None, ALU.mult, r=[self.Ab], w=[self.Ab])
        for l in range(L):
            for k in range(4):
                self.dma(self.sconv.t[:, l, :, k], d["ssd_conv_w"][l, k].rearrange("(c p) -> p c", p=128), w=[self.sconv], slow=True)
            self.dma(self.sconvb.t[:, l, :], d["ssd_conv_b"][l].rearrange("(c p) -> p c", p=128), w=[self.sconvb], slow=True)
            for k in range(3):
                self.dma(self.fconv.t[:, l, :, k], d["ffn_conv_w"][l, k].rearrange("(c p) -> p c", p=128), w=[self.fconv], slow=True)
            self.dma(self.fconvb.t[:, l, :], d["ffn_conv_b"][l].rearrange("(c p) -> p c", p=128), w=[self.fconvb], slow=True)
        p.pool(lambda e: e.memset(self.w2k.t[:, :, :, :], 0.0), w=[self.w2k])
        m0 = self.ar.off
        self.prologue()
        p.dve(lambda e: e.memset(self.junkb.t[:, 0:1], 0.0), r=[self.scr] + self.pro_res, w=[self.scr, self.junkb] + self.xTr)
        self.ar.off = m0
        xs = lambda tt: d["xs_s"][tt * 128:(tt + 1) * 128, :]
        for s in range(self.nseq):
            xin = lambda tt, s=s: d["x"][s, tt * 128:(tt + 1) * 128, :]
            for l in range(self.nlayer):
                src = xin if l == 0 else xs
                import os as _os
                ph = _os.environ.get("MKPH", "ssd,nsa,ffn").split(",")
                self.norm_T(src)
                if "ssd" in ph:
                    self.ssd_phase(l, src, xs)
                if "nsa" in ph:
                    self.nsa_phase(l, xs, xs)
                self.norm_T(xs)
                if "ffn" in ph:
                    self.ffn_phase(l, xs, xs)
            for tt in range(NT):
                xt = self.xtile[tt % 2]
                self.dma(xt.t[:, :], xs(tt), r=self.xsr[tt], w=[xt], q=XQ)
                st = self.nst[tt % 2]
                p.dve(lambda e, o_=st.t[:, 0:1]: e.memset(o_, 0.0), w=[st])
                self.actf(self.junkb.t[:, :], xt.t[:, :], AF.Square, r=[xt, st], w=[self.junkb, st], accum=st.t[:, 0:1])
                self.rms_scale(st.t[:, 0:1], st.t[:, 1:2], D, r=[st], w=[st])
                self.ts(xt.t[:, :], xt.t[:, :], st.t[:, 1:2], None, ALU.mult, r=[xt, st], w=[xt])
                self.tt(xt.t[:, :], xt.t[:, :], self.nfw.t[:, :], ALU.mult, r=[xt, self.nfw], w=[xt])
                self.p.dma(d["out"][s, tt * 128:(tt + 1) * 128, :], xt.t[:, :], r=[xt], w=[], q=XQ, out_final=True)
        p.finish()
        return nc


_CACHE = {}


def kernel(**inputs):
    ncores = 8
    nseq = 4
    key = (nseq, L)
    if key not in _CACHE:
        _CACHE[key] = K(nseq, L).build()
    nc = _CACHE[key]
    consts = host_consts()
    x = np.ascontiguousarray(inputs["x"], dtype=np.float32)
    in_maps = []
    for c in range(ncores):
        m = {"x": np.ascontiguousarray(x[c * nseq:(c + 1) * nseq])}
        for k in WEIGHT_SHAPES:
            m[k] = np.ascontiguousarray(np.asarray(inputs[k], dtype=np.float32))
        m.update(consts)
        in_maps.append(m)
    res = run_bass_kernel_spmd(nc, in_maps, core_ids=list(range(ncores)))
    return np.concatenate([np.asarray(r["out"]) for r in res.results], axis=0).astype(np.float32)
```
